# Optimizing a Trainium2 kernel written in Bass

```python
import jax, jax.numpy as jnp
from jax import lax
import numpy as np

D_MODEL = 1024
BATCH = 16
SEQ = 2048
DEPTH = 2

GRID_W = 64
CTX_LEN = 256

N_EVEN = (DEPTH + 1) // 2
N_ODD = DEPTH // 2

N_HEADS = 8
Q_LORA = 384
KV_LORA = 256
NOPE_DIM = 64
ROPE_DIM = 32
QK_DIM = NOPE_DIM + ROPE_DIM
V_DIM = 64
ROPE_THETA = 10000.0
Q_BLOCK = 128

CONV_CH = 512
CONV_W = 31

A_IN = Q_LORA + KV_LORA + ROPE_DIM + 2 * CONV_CH
MIX_WIDTH = N_HEADS * V_DIM + CONV_CH

SG_DIM = 2 * D_MODEL
CHUNK = 128
SG_GROUPS = 8
SG_CH = SG_DIM // SG_GROUPS

N_GROUPS = 4
EXPERTS_PER_GROUP = 8
N_EXPERTS = N_GROUPS * EXPERTS_PER_GROUP
TOP_K = 2
D_EXPERT = 256

kernel_name = "hybrid_mla_conformer_gmlp_hmoe_dit"


def rms_norm(x, g, eps=1e-6):
    xf = x.astype(jnp.float32)
    y = xf * lax.rsqrt(jnp.mean(xf * xf, axis=-1, keepdims=True) + eps)
    return (y * g.astype(jnp.float32)).astype(x.dtype)


def layer_norm(x, g, b, eps=1e-5):
    xf = x.astype(jnp.float32)
    mu = jnp.mean(xf, axis=-1, keepdims=True)
    var = jnp.mean(jnp.square(xf - mu), axis=-1, keepdims=True)
    y = (xf - mu) * lax.rsqrt(var + eps)
    return (y * g.astype(jnp.float32) + b.astype(jnp.float32)).astype(x.dtype)


def modulate(h, shift, scale):
    return h * (1 + scale) + shift


def axial_rope(n_tokens, dtype):
    rows = n_tokens // GRID_W
    t = jnp.arange(rows * GRID_W)
    row = (t // GRID_W).astype(jnp.float32)
    col = (t % GRID_W).astype(jnp.float32)
    n_freq = ROPE_DIM // 4
    inv_freq = ROPE_THETA ** (-jnp.arange(n_freq, dtype=jnp.float32) / n_freq)
    ang = jnp.concatenate([row[:, None] * inv_freq, col[:, None] * inv_freq], axis=-1)
    return jnp.cos(ang).astype(dtype), jnp.sin(ang).astype(dtype)


def rope_tail(x, cos, sin):
    half = ROPE_DIM // 2
    xn = x[..., :NOPE_DIM]
    x1 = x[..., NOPE_DIM:NOPE_DIM + half]
    x2 = x[..., NOPE_DIM + half:]
    cs = cos[None, :, None, :]
    sn = sin[None, :, None, :]
    return jnp.concatenate([xn, x1 * cs - x2 * sn, x1 * sn + x2 * cs], axis=-1)


def mla_queries(q_lat, q_norm_g, w_uq, q_g, rope):
    q = rms_norm(q_lat, q_norm_g) @ w_uq
    q = q.reshape(q.shape[:-1] + (N_HEADS, QK_DIM))
    q = rms_norm(q, q_g)
    if rope is not None:
        q = rope_tail(q, *rope)
    return q


def mla_keys_values(kv_lat, k_rope, kv_norm_g, w_ukv, k_g, rope):
    kv = rms_norm(kv_lat, kv_norm_g) @ w_ukv
    kv = kv.reshape(kv.shape[:-1] + (N_HEADS, NOPE_DIM + V_DIM))
    k_nope, v = kv[..., :NOPE_DIM], kv[..., NOPE_DIM:]
    kr = jnp.broadcast_to(k_rope[..., None, :], k_nope.shape[:-1] + (ROPE_DIM,))
    k = rms_norm(jnp.concatenate([k_nope, kr], axis=-1), k_g)
    if rope is not None:
        k = rope_tail(k, *rope)
    return k, v


def attend(q, k, v):
    s = jnp.einsum('bqhd,bkhd->bhqk', q, k).astype(jnp.float32) * (QK_DIM ** -0.5)
    p = jax.nn.softmax(s, axis=-1).astype(v.dtype)
    return jnp.einsum('bhqk,bkhd->bqhd', p, v)


def latent_attention(q, k_all, v_all):
    B, S = q.shape[0], q.shape[1]
    nb = S // Q_BLOCK
    qb = q.reshape(B, nb, Q_BLOCK, N_HEADS, QK_DIM).transpose(1, 0, 2, 3, 4)
    o = lax.map(lambda qblk: attend(qblk, k_all, v_all), qb)
    return o.transpose(1, 0, 2, 3, 4).reshape(B, S, N_HEADS * V_DIM)


def conformer_conv(g, w_dw, b_dw, ln_g, ln_b):
    a, b = jnp.split(g, 2, axis=-1)
    z = a * jax.nn.sigmoid(b)
    z = lax.conv_general_dilated(
        z, w_dw[:, None, :].astype(z.dtype), window_strides=(1,),
        padding=[(CONV_W // 2, CONV_W // 2)],
        dimension_numbers=('NWC', 'WIO', 'NWC'), feature_group_count=CONV_CH) + b_dw
    return jax.nn.silu(layer_norm(z, ln_g, ln_b))


def mla_conv_mixer(h, hc, w_in, q_norm_g, w_uq, kv_norm_g, w_ukv, q_g, k_g,
                   w_dw, b_dw, ln_g, ln_b, w_out, ctx_out):
    S = h.shape[1]
    splits = [Q_LORA, Q_LORA + KV_LORA, Q_LORA + KV_LORA + ROPE_DIM]
    q_lat, kv_lat, kr_lat, glu_lat = jnp.split(h @ w_in, splits, axis=-1)
    rope = axial_rope(S, h.dtype)
    q = mla_queries(q_lat, q_norm_g, w_uq, q_g, rope)
    k, v = mla_keys_values(kv_lat, kr_lat, kv_norm_g, w_ukv, k_g, rope)
    if ctx_out:
        q_c_lat, kv_c, kr_c, glu_c = jnp.split(hc @ w_in, splits, axis=-1)
    else:
        kv_c, kr_c = jnp.split(hc @ w_in[:, Q_LORA:Q_LORA + KV_LORA + ROPE_DIM], [KV_LORA], axis=-1)
    k_c, v_c = mla_keys_values(kv_c, kr_c, kv_norm_g, w_ukv, k_g, None)
    k_all = jnp.concatenate([k_c, k], axis=1)
    v_all = jnp.concatenate([v_c, v], axis=1)
    attn = latent_attention(q, k_all, v_all)
    conv = conformer_conv(glu_lat, w_dw, b_dw, ln_g, ln_b)
    y = jnp.concatenate([attn, conv], axis=-1) @ w_out
    if not ctx_out:
        return y, None
    q_c = mla_queries(q_c_lat, q_norm_g, w_uq, q_g, None)
    attn_c = attend(q_c, k_c, v_c).reshape(hc.shape[0], hc.shape[1], N_HEADS * V_DIM)
    conv_c = conformer_conv(glu_c, w_dw, b_dw, ln_g, ln_b)
    yc = jnp.concatenate([attn_c, conv_c], axis=-1) @ w_out
    return y, yc


def chunk_gating_mlp(h, w_in, b_in, ln_g, ln_b, w_s, b_s, w_out):
    B, N, _ = h.shape
    z = jax.nn.gelu(h @ w_in + b_in)
    u, v = jnp.split(z, 2, axis=-1)
    v = layer_norm(v, ln_g, ln_b).reshape(B, N // CHUNK, CHUNK, SG_GROUPS, SG_CH)
    s = jnp.einsum('gpq,bnqgc->bnpgc', w_s, v) + b_s.T[:, :, None]
    return (u * s.reshape(B, N, SG_DIM)) @ w_out


def hier_moe(h, w_group, b_group, w_router, b_router, w_gate, w_up, w_down):
    shp = h.shape
    t = h.reshape(-1, shp[-1])
    gl = (t @ w_group + b_group).astype(jnp.float32)
    gp = jax.nn.softmax(gl, axis=-1)
    gidx = jnp.argmax(gl, axis=-1)
    gw = jnp.take_along_axis(gp, gidx[:, None], axis=-1)
    el = (jnp.einsum('td,gde->tge', t, w_router) + b_router).astype(jnp.float32)
    el = jnp.take_along_axis(el, gidx[:, None, None], axis=1)[:, 0]
    tv, ti = lax.top_k(el, TOP_K)
    ew = jax.nn.softmax(tv, axis=-1) * gw
    eid = gidx[:, None] * EXPERTS_PER_GROUP + ti
    dense_w = jnp.sum(jax.nn.one_hot(eid, N_EXPERTS, dtype=jnp.float32) * ew[..., None], axis=1)
    dense_w = dense_w.astype(t.dtype)
    out = jnp.zeros_like(t)
    for e in range(N_EXPERTS):
        he = (jax.nn.silu(t @ w_gate[e]) * (t @ w_up[e])) @ w_down[e]
        out = out + dense_w[:, e:e + 1] * he
    return out.reshape(shp)


def setup_inputs(seed: int = 0) -> dict:
    key = jax.random.key(seed)
    ks = iter(jax.random.split(key, 40))

    def nrm(shape, scale):
        return jax.random.normal(next(ks), shape, jnp.float32) * scale

    def gain(shape):
        return 1.0 + nrm(shape, 0.02)

    D = D_MODEL
    NE, NO = N_EVEN, N_ODD
    return {
        "x": nrm((BATCH, SEQ, D), 1.0),
        "c": nrm((BATCH, D), 1.0),
        "ctx": nrm((BATCH, CTX_LEN, D), 1.0),
        "c_ctx": nrm((D,), 1.0),
        "w_ada": nrm((DEPTH, D, 6 * D), 0.5 * D ** -0.5),
        "b_ada": nrm((DEPTH, 6 * D), 0.02),
        "norm1_g": gain((DEPTH, D)),
        "norm2_g": gain((DEPTH, D)),
        "a_w_in": nrm((NE, D, A_IN), D ** -0.5),
        "a_q_norm_g": gain((NE, Q_LORA)),
        "a_w_uq": nrm((NE, Q_LORA, N_HEADS * QK_DIM), Q_LORA ** -0.5),
        "a_kv_norm_g": gain((NE, KV_LORA)),
        "a_w_ukv": nrm((NE, KV_LORA, N_HEADS * (NOPE_DIM + V_DIM)), KV_LORA ** -0.5),
        "a_q_g": gain((NE, QK_DIM)),
        "a_k_g": gain((NE, QK_DIM)),
        "b_w_dw": nrm((NE, CONV_W, CONV_CH), CONV_W ** -0.5),
        "b_b_dw": nrm((NE, CONV_CH), 0.02),
        "b_ln_g": gain((NE, CONV_CH)),
        "b_ln_b": nrm((NE, CONV_CH), 0.02),
        "ab_w_out": nrm((NE, MIX_WIDTH, D), MIX_WIDTH ** -0.5),
        "c_w_in": nrm((NO, D, 2 * SG_DIM), D ** -0.5),
        "c_b_in": nrm((NO, 2 * SG_DIM), 0.02),
        "c_ln_g": gain((NO, SG_DIM)),
        "c_ln_b": nrm((NO, SG_DIM), 0.02),
        "c_w_s": nrm((NO, SG_GROUPS, CHUNK, CHUNK), CHUNK ** -0.5),
        "c_b_s": gain((NO, SG_GROUPS, CHUNK)),
        "c_w_out": nrm((NO, SG_DIM, D), SG_DIM ** -0.5),
        "moe_w_group": nrm((DEPTH, D, N_GROUPS), D ** -0.5),
        "moe_b_group": nrm((DEPTH, N_GROUPS), 0.01),
        "moe_w_router": nrm((DEPTH, N_GROUPS, D, EXPERTS_PER_GROUP), D ** -0.5),
        "moe_b_router": nrm((DEPTH, N_GROUPS, EXPERTS_PER_GROUP), 0.01),
        "moe_w_gate": nrm((DEPTH, N_EXPERTS, D, D_EXPERT), D ** -0.5),
        "moe_w_up": nrm((DEPTH, N_EXPERTS, D, D_EXPERT), D ** -0.5),
        "moe_w_down": nrm((DEPTH, N_EXPERTS, D_EXPERT, D), D_EXPERT ** -0.5),
    }


def reference(x, c, ctx, c_ctx, w_ada, b_ada, norm1_g, norm2_g,
              a_w_in, a_q_norm_g, a_w_uq, a_kv_norm_g, a_w_ukv, a_q_g, a_k_g,
              b_w_dw, b_b_dw, b_ln_g, b_ln_b, ab_w_out,
              c_w_in, c_b_in, c_ln_g, c_ln_b, c_w_s, c_b_s, c_w_out,
              moe_w_group, moe_b_group, moe_w_router, moe_b_router,
              moe_w_gate, moe_w_up, moe_w_down):
    xc = ctx
    silu_c = jax.nn.silu(c)
    silu_cc = jax.nn.silu(c_ctx)
    for l in range(DEPTH):
        i = l // 2
        ctx_later = any(j % 2 == 0 for j in range(l + 1, DEPTH))
        ctx_here = (l % 2 == 0) or ctx_later
        mod = (silu_c @ w_ada[l] + b_ada[l])[:, None, :]
        sh1, sc1, g1, sh2, sc2, g2 = jnp.split(mod, 6, axis=-1)
        h = modulate(rms_norm(x, norm1_g[l]), sh1, sc1)
        hc = None
        if ctx_here:
            mod_c = silu_cc @ w_ada[l] + b_ada[l]
            csh1, csc1, cg1, csh2, csc2, cg2 = jnp.split(mod_c, 6, axis=-1)
            hc = modulate(rms_norm(xc, norm1_g[l]), csh1, csc1)
        if l % 2 == 0:
            y, yc = mla_conv_mixer(h, hc, a_w_in[i], a_q_norm_g[i], a_w_uq[i], a_kv_norm_g[i],
                                   a_w_ukv[i], a_q_g[i], a_k_g[i], b_w_dw[i], b_b_dw[i],
                                   b_ln_g[i], b_ln_b[i], ab_w_out[i], ctx_later)
        else:
            y = chunk_gating_mlp(h, c_w_in[i], c_b_in[i], c_ln_g[i], c_ln_b[i],
                                 c_w_s[i], c_b_s[i], c_w_out[i])
            yc = None
            if ctx_later:
                yc = chunk_gating_mlp(hc, c_w_in[i], c_b_in[i], c_ln_g[i], c_ln_b[i],
                                      c_w_s[i], c_b_s[i], c_w_out[i])
        x = x + g1 * y
        moe_args = (moe_w_group[l], moe_b_group[l], moe_w_router[l], moe_b_router[l],
                    moe_w_gate[l], moe_w_up[l], moe_w_down[l])
        h2 = modulate(rms_norm(x, norm2_g[l]), sh2, sc2)
        x = x + g2 * hier_moe(h2, *moe_args)
        if ctx_later:
            xc = xc + cg1 * yc
            hc2 = modulate(rms_norm(xc, norm2_g[l]), csh2, csc2)
            xc = xc + cg2 * hier_moe(hc2, *moe_args)
    return x
```

```python
import contextlib
import math
import numpy as np
import concourse.bass as bass
import concourse.mybir as mybir
from concourse.bass_utils import run_bass_kernel_spmd

F32 = mybir.dt.float32
BF16 = mybir.dt.bfloat16
AF = mybir.ActivationFunctionType
ALU = mybir.AluOpType
AX = mybir.AxisListType

D = 1024
S = 2048
CTX = 256
NCORES = 8
NB = 2

V_C, V_BADA, V_N1G, V_N2G, V_QNG, V_KVNG, V_BLNG, V_BLNB, V_CBU = 0, 24, 120, 136, 152, 155, 157, 161, 165
V_DW = 181
NV = 305
R_BDW, R_CBV, R_BS, R_MOEB = 0, 512, 2560, 3584
NR = 3656
ARENA = 53000
STRICT_SAME_ENGINE = False


class Prog:
    ENGS = ("pe", "act", "dve", "pool", "sp")
    N_DMA_SEMS = 16

    def __init__(self, nc):
        self.nc = nc
        self.ops = []
        self.last_w = {}
        self.readers = {}

    def op(self, eng, fn, reads=(), writes=(), dma=False, semq=None):
        deps = {}
        for k in reads:
            w = self.last_w.get(k)
            if w is not None:
                deps[w] = True
        for k in writes:
            w = self.last_w.get(k)
            if w is not None:
                deps.setdefault(w, False)
            for r in self.readers.get(k, ()):
                deps.setdefault(r, False)
        i = len(self.ops)
        self.ops.append(dict(eng=eng, fn=fn, deps=deps, dma=dma, barrier=False, semq=semq))
        for k in reads:
            self.readers.setdefault(k, []).append(i)
        for k in writes:
            self.last_w[k] = i
            self.readers[k] = []
        return i

    def barrier(self):
        self.ops.append(dict(eng=None, fn=None, deps={}, dma=False, barrier=True))

    def emit(self):
        nc = self.nc
        ops = self.ops
        signal = [False] * len(ops)
        for i, o in enumerate(ops):
            if o["barrier"]:
                continue
            enf = []
            for d, raw in o["deps"].items():
                od = ops[d]
                if od["dma"]:
                    enf.append(d)
                    continue
                if od["eng"] == o["eng"] and not o["dma"]:
                    if o["eng"] == "pe" or (not raw and not STRICT_SAME_ENGINE):
                        continue
                enf.append(d)
                signal[d] = True
            o["enf"] = enf
        last = {}
        for i, o in enumerate(ops):
            if o["barrier"]:
                o["last"] = dict(last)
                for e, j in last.items():
                    signal[j] = True
            elif not o["dma"]:
                last[o["eng"]] = i
        cnt = {e: 0 for e in self.ENGS}
        for i, o in enumerate(ops):
            if o["barrier"] or o["dma"]:
                continue
            if signal[i]:
                cnt[o["eng"]] += 1
                o["sig"] = cnt[o["eng"]]
        with contextlib.ExitStack() as st:
            sems = {e: st.enter_context(nc.semaphore("s_" + e)) for e in ("pe", "act", "dve", "pool")}
            nsem = {"sp": self.N_DMA_SEMS, "act": self.N_DMA_SEMS, "pool": self.N_DMA_SEMS, "bg": 28}
            dsems = {q: [st.enter_context(nc.semaphore(f"d_{q}{j}")) for j in range(n)] for q, n in nsem.items()}
            dcount = {q: 0 for q in dsems}
            dtarget = {(q, j): 0 for q in dsems for j in range(nsem[q])}
            for i, o in enumerate(ops):
                if o["barrier"]:
                    o["dsnap"] = {k_: v_ for k_, v_ in dtarget.items() if k_[0] != "bg"}
                elif o["dma"]:
                    q = o.get("semq") or o["eng"]
                    j = dcount[q] % nsem[q]
                    dcount[q] += 1
                    o["dq"] = (q, j)
                    o["dprev"] = dtarget[(q, j)]
                    dtarget[(q, j)] += 16
                    o["dtgt"] = dtarget[(q, j)]
            block = st.enter_context(nc.Block())

            def stream(engname):
                def body(e):
                    seen = {}
                    mydma = {}

                    def wait(key, sem, val):
                        if val > 0 and seen.get(key, 0) < val:
                            e.wait_ge(sem, val)
                            seen[key] = val

                    for i, o in enumerate(ops):
                        if o["barrier"]:
                            for en, j in o["last"].items():
                                if en != engname:
                                    wait(en, sems[en], ops[j]["sig"])
                            for (q, j), t in o["dsnap"].items():
                                wait((q, j), dsems[q][j], t)
                            continue
                        if o["eng"] != engname:
                            continue
                        for d in o["enf"]:
                            od = ops[d]
                            if od["dma"]:
                                q, j = od["dq"]
                                wait((q, j), dsems[q][j], od["dtgt"])
                            else:
                                wait(od["eng"], sems[od["eng"]], od["sig"])
                        if o["dma"]:
                            q, j = o["dq"]
                            wait((q, j), dsems[q][j], o["dprev"])
                            ins = o["fn"](e)
                            ins.then_inc(dsems[q][j], 16)
                            mydma[(q, j)] = o["dtgt"]
                        else:
                            ins = o["fn"](e)
                            if signal[i]:
                                ins.then_inc(sems[engname], 1)
                    for (q, j), t in mydma.items():
                        wait((q, j), dsems[q][j], t)
                return body

            used = {o["eng"] for o in ops if not o["barrier"]}
            if "pe" in used:
                block.tensor(stream("pe"))
            if "act" in used:
                block.scalar(stream("act"))
            if "dve" in used:
                block.vector(stream("dve"))
            if "pool" in used:
                block.gpsimd(stream("pool"))
            if "sp" in used:
                block.sync(stream("sp"))


def MM(out, lhsT, rhs, start, stop):
    return lambda e: e.matmul(out, lhsT=lhsT, rhs=rhs, start=start, stop=stop)


def TR(out, in_, ident):
    return lambda e: e.transpose(out=out, in_=in_, identity=ident)


def ACTV(out, in_, func, bias=None, scale=None):
    kw = {}
    if bias is not None:
        kw["bias"] = bias
    if scale is not None:
        kw["scale"] = scale
    return lambda e: e.activation(out=out, in_=in_, func=func, **kw)


def TT(out, in0, in1, op):
    return lambda e: e.tensor_tensor(out=out, in0=in0, in1=in1, op=op)


def TS(out, in0, s1, s2, op0, op1):
    return lambda e: e.tensor_scalar(out=out, in0=in0, scalar1=s1, scalar2=s2, op0=op0, op1=op1)


def TS1(out, in_, s, op):
    return lambda e: e.tensor_single_scalar(out=out, in_=in_, scalar=s, op=op)


def STT(out, in0, scalar, in1, op0, op1):
    return lambda e: e.scalar_tensor_tensor(out=out, in0=in0, scalar=scalar, in1=in1, op0=op0, op1=op1)


def CP(out, in_):
    return lambda e: e.tensor_copy(out=out, in_=in_)


def DMA(out, in_):
    return lambda e: e.dma_start(out=out, in_=in_)


def RMAX(out, in_):
    return lambda e: e.reduce_max(out=out, in_=in_, axis=AX.X)


def RSUM(out, in_):
    return lambda e: e.reduce_sum(out=out, in_=in_, axis=AX.X)


def MSET(ap, v):
    return lambda e: e.memset(ap, v)


class _Rec:
    def __init__(self):
        self.l = []

    def op(self, *a, **k):
        self.l.append((a, k))


class KB:
    def __init__(self, phases, nb=NB, debug=None):
        self.phases = phases
        self.nb = nb
        nc = self.nc = bass.Bass("TRN2", target_bir_lowering=False)
        self.st = contextlib.ExitStack()
        din = lambda name, shape: nc.dram_tensor(name, list(shape), F32, kind="ExternalInput").ap()
        self.sparse = any(p.startswith("smoe") for p in phases)
        self.d = dict(
            xT=din("xT", [nb, D, S]), ctxT=din("ctxT", [nb, D, CTX]), vecs=din("vecs", [128, NV]),
            rows=din("rows", [1, NR]), rope=din("rope", [128, 16, 32]), ident=din("ident", [128, 128]),
            w_ada=din("w_ada", [2, D, 6 * D]), a_w_in=din("a_w_in", [D, 1696]), a_w_uq=din("a_w_uq", [384, 768]),
            a_w_ukv=din("a_w_ukv", [256, 1024]), a_q_g=din("a_q_g", [1, 96]), a_k_g=din("a_k_g", [1, 96]),
            b_w_dw=din("b_w_dw", [31, 512]), ab_w_out=din("ab_w_out", [D, D]),
            c_w_in=din("c_w_in", [D, 4096]), c_ln_g=din("c_ln_g", [1, 2048]), c_ln_b=din("c_ln_b", [1, 2048]),
            w_sT=din("w_sT", [128, 8, 128]), c_w_out=din("c_w_out", [2048, D]),
            wr=din("wr", [2, D, 36]),
        )
        if self.sparse:
            self.d.update(consts=din("consts", [128, 193]),
                          wgu_t=[din(f"wgu_t{i}", [4096, 4096]) for i in range(2)],
                          wd_t=[din(f"wd_t{i}", [4096, 2048]) for i in range(2)])
            self.hs = nc.dram_tensor("hs_scr", [S, D], BF16, kind="Internal").ap()
            self.SL = nc.dram_tensor("sl_scr", [8192, 4], F32, kind="Internal").ap()
            self.Y = nc.dram_tensor("y_scr", [2 * S, D], F32, kind="Internal").ap()
            self.wgu_b = [nc.dram_tensor(f"wgu_b{i}", [4096, 4096], BF16, kind="Internal").ap() for i in range(2)]
            self.wd_b = [nc.dram_tensor(f"wd_b{i}", [4096, 2048], BF16, kind="Internal").ap() for i in range(2)]
            self.cwin_b = nc.dram_tensor("cwin_b", [D, 4096], BF16, kind="Internal").ap()
            self.cwout_b = nc.dram_tensor("cwout_b", [2048, D], BF16, kind="Internal").ap()
            self.cast_jobs = [
                (0, self.cwin_b[0:512, :], self.d["c_w_in"][0:512, :], ("cwcast", 0)),
                (0, self.cwin_b[512:1024, :], self.d["c_w_in"][512:1024, :], ("cwcast", 1)),
                (0, self.cwout_b[0:1024, :], self.d["c_w_out"][0:1024, :], ("cwcast", 2)),
                (0, self.cwout_b[1024:2048, :], self.d["c_w_out"][1024:2048, :], ("cwcast", 3)),
            ]
            for i in range(2):
                for q in range(8):
                    self.cast_jobs.append((i, self.wgu_b[i][q * 512:(q + 1) * 512, :], self.d["wgu_t"][i][q * 512:(q + 1) * 512, :], ("wcast", i, q)))
                for q in range(4):
                    self.cast_jobs.append((i, self.wd_b[i][q * 1024:(q + 1) * 1024, :], self.d["wd_t"][i][q * 1024:(q + 1) * 1024, :], ("wcast", i, 8 + q)))
        else:
            self.d.update(moe_w_gate=din("moe_w_gate", [2, 32, D, 256]), moe_w_up=din("moe_w_up", [2, 32, D, 256]),
                          moe_w_down=din("moe_w_down", [2, 32, 256, D]))
        self.outT = nc.dram_tensor("outT", [nb, D, S], F32, kind="ExternalOutput").ap()
        self.P = Prog(nc)
        self._n = 0
        self.build()

    def sb(self, shape, dt, name=None):
        self._n += 1
        return self.st.enter_context(self.nc.sbuf_tensor("sb_" + (name or f"t{self._n}"), list(shape), dt))

    def av(self, off, shape, dt=BF16, parts=128):
        n = int(np.prod(shape))
        if dt == F32:
            ap = self.arena[0:parts, off:off + 2 * n].bitcast(F32)
        else:
            ap = self.arena[0:parts, off:off + n]
        assert off + (2 * n if dt == F32 else n) <= ARENA, (off, shape)
        if len(shape) == 2:
            return ap.rearrange("p (a b) -> p a b", a=shape[0])
        if len(shape) == 3:
            return ap.rearrange("p (a b c) -> p a b c", a=shape[0], b=shape[1])
        return ap

    def build(self):
        nc, P, d = self.nc, self.P, self.d
        with self.st:
            self.xT = self.sb([128, 8, S], F32, "xT")
            self.arena = self.sb([128, ARENA], BF16, "arena")
            self.psall = self.st.enter_context(nc.psum_tensor("psall", [128, 4096], F32))
            self.ps = [self.psall[:, i * 512:(i + 1) * 512] for i in range(8)]
            self.vecs = self.sb([128, NV], F32, "vecs")
            self.modT = self.sb([128, 2, 48, 3], F32, "modT")
            self.scl1 = self.sb([128, 2, 8, 3], F32, "scl1")
            self.scl2 = self.sb([128, 2, 8, 3], F32, "scl2")
            self.ident_f = self.sb([128, 128], F32, "ident_f")
            self.ident_b = self.sb([128, 128], BF16, "ident_b")
            self.ones_b = self.sb([128, 128], BF16, "ones_b")
            self.ones_f = self.sb([1, 128], F32, "ones_f")
            self.kfin = self.sb([128, 4, 96], BF16, "kfin")
            self.qfin = self.sb([128, 4, 96], BF16, "qfin")
            self.rows_f = self.sb([1, 72], F32, "rows_f")
            self.rope = self.sb([128, 16, 32], F32, "rope")
            self.sel = self.sb([32, 32, 128], BF16, "sel")
            self.qgB = self.sb([128, 96], F32, "qgB")
            self.kgB = self.sb([128, 96], F32, "kgB")
            self.qkng = self.sb([128, 5], F32, "qkng")
            self.negC = self.sb([128, 1], F32, "negC")
            self.wr = self.sb([128, 2, 8, 36], F32, "wr")
            self.scT = self.sb([128, 24], BF16, "scT")
            self.sq = [self.sb([128, 512], BF16, f"sq{i}") for i in range(2)]
            self.rstdB = self.sb([128, 512], F32, "rstdB")
            self.tmpf = [self.sb([128, 512], F32, f"tmpf{i}") for i in range(2)]
            self.tmpb2 = [self.sb([128, 1024], BF16, f"tmpb2_{i}") for i in range(2)]
            self.tmpb = [self.tmpb2[i // 2][:, (i % 2) * 512:(i % 2 + 1) * 512] for i in range(4)]
            self.sm = self.sb([128, 24, 36], F32, "sm")
            self.cst = self.sb([128, 8], F32, "cst")
            if self.sparse:
                self.iota = self.sb([128, 65], F32, "iota")
                self.U_b = self.sb([128, 128], BF16, "U_b")
            self._cnt = {}
            self.setup()
            for bi in range(self.nb):
                self.load_x(bi)
                for ph in self.phases:
                    P.barrier()
                    if ph == "mix0":
                        self.phase_mix0(bi)
                    elif ph == "moe0":
                        self.phase_moe(0, bi)
                    elif ph == "mix1":
                        self.phase_mix1(bi)
                    elif ph == "moe1":
                        self.phase_moe(1, bi)
                    elif ph == "smoe0":
                        self.phase_smoe(0, bi)
                    elif ph == "smoe1":
                        self.phase_smoe(1, bi)
                P.barrier()
                self.store_x(bi)
            P.emit()

    def issue_casts(self, n=None, layer=None):
        if not self.sparse:
            return
        k = 0
        while self.cast_jobs and (n is None or k < n):
            if layer is not None and self.cast_jobs[0][0] > layer:
                break
            i, dst, src, key = self.cast_jobs.pop(0)
            self.P.op("pool", DMA(dst, src), writes=[key], dma=True, semq="bg")
            k += 1

    def rot(self, name, n):
        c = self._cnt.get(name, 0)
        self._cnt[name] = c + 1
        return c % n

    def setup(self):
        P, d = self.P, self.d
        P.op("sp", DMA(self.vecs[:], d["vecs"]), writes=["vecs"], dma=True)
        P.op("sp", DMA(self.ident_f[:], d["ident"]), writes=["ident_f"], dma=True)
        P.op("pool", DMA(self.ident_b[:], d["ident"]), writes=["ident_b"], dma=True)
        P.op("sp", DMA(self.rows_f[:], d["rows"][:, R_MOEB:R_MOEB + 72]), writes=["rows_f"], dma=True)
        P.op("sp", DMA(self.rope[:], d["rope"]), writes=["rope"], dma=True)
        P.op("sp", DMA(self.qgB[:], d["a_q_g"][0].partition_broadcast(128)), writes=["qgB"], dma=True)
        P.op("sp", DMA(self.kgB[:], d["a_k_g"][0].partition_broadcast(128)), writes=["kgB"], dma=True)
        for l in range(2):
            P.op("sp", DMA(self.wr[:, l], d["wr"][l].rearrange("(kc p) n -> p kc n", p=128)), writes=["wr"], dma=True)
        if self.sparse:
            P.op("sp", DMA(self.iota[:], d["consts"][:, 0:65]), writes=["iota"], dma=True)
            P.op("pool", DMA(self.U_b[:], d["consts"][:, 65:193]), writes=["U_b"], dma=True)
        P.op("dve", MSET(self.ones_b[:], 1.0), writes=["ones_b"])
        for ci, v in enumerate((1024e-6, 384e-6, 256e-6, 96e-6, 1e-5, 0.0)):
            P.op("dve", MSET(self.cst[:, ci:ci + 1], v), writes=["cst"])
        P.op("dve", MSET(self.ones_f[:], 1.0), writes=["ones_f"])
        P.op("dve", CP(self.sel[:], self.ident_b[0:32, 0:32].unsqueeze(2).broadcast_to([32, 32, 128])),
             reads=["ident_b"], writes=["sel"])
        P.op("act", ACTV(self.scT[:], self.vecs[:, V_C:V_C + 24], AF.Silu), reads=["vecs"], writes=["scT"])
        psm = self.ps[0]
        for l in range(2):
            for s in range(12):
                slot = self.rot("wada", 4)
                wb = self.av(slot * 4096, [8, 512])
                P.op("pool", DMA(wb, self.d["w_ada"][l][:, s * 512:(s + 1) * 512].rearrange("(kc p) n -> p kc n", p=128)),
                     writes=[("wada", slot)], dma=True)
                for q in range(4):
                    ch = s * 4 + q
                    for kc in range(8):
                        P.op("pe", MM(psm[:, ch * 3:ch * 3 + 3], wb[:, kc, q * 128:(q + 1) * 128],
                                      self.scT[:, kc * 3:kc * 3 + 3], kc == 0, kc == 7),
                             reads=[("wada", slot), "scT"], writes=["ps0"])
            P.op("dve", TT(self.modT[:, l], psm[:, 0:144].rearrange("p (a b) -> p a b", a=48),
                           self.vecs[:, V_BADA + l * 48:V_BADA + (l + 1) * 48].unsqueeze(2).broadcast_to([128, 48, 3]),
                           ALU.add), reads=["ps0", "vecs"], writes=["modT"])
            for (scl, c0, vg) in ((self.scl1, 8, V_N1G), (self.scl2, 32, V_N2G)):
                P.op("dve", TS(scl[:, l], self.modT[:, l, c0:c0 + 8, :], 1.0, 32.0, ALU.add, ALU.mult),
                     reads=["modT"], writes=["scl"])
                P.op("dve", TT(scl[:, l], scl[:, l],
                               self.vecs[:, vg + l * 8:vg + (l + 1) * 8].unsqueeze(2).broadcast_to([128, 8, 3]), ALU.mult),
                     reads=["scl", "vecs"], writes=["scl"])
        P.op("dve", TS1(self.qkng[:, 0:3], self.vecs[:, V_QNG:V_QNG + 3], math.sqrt(384.0), ALU.mult),
             reads=["vecs"], writes=["qkng"])
        P.op("dve", TS1(self.qkng[:, 3:5], self.vecs[:, V_KVNG:V_KVNG + 2], 16.0, ALU.mult),
             reads=["vecs", "qkng"], writes=["qkng"])
        t = self.sm
        P.op("dve", TT(self.tmpf[0][:, 0:96], self.qgB[:], self.qgB[:], ALU.mult),
             reads=["qgB"], writes=["tmpf0"])
        P.op("dve", RMAX(t[:, 0, 0:1], self.tmpf[0][:, 0:96]), reads=["tmpf0"], writes=["sm0"])
        P.op("dve", TT(self.tmpf[1][:, 0:96], self.kgB[:], self.kgB[:], ALU.mult), reads=["kgB"], writes=["tmpf1"])
        P.op("dve", RMAX(t[:, 0, 1:2], self.tmpf[1][:, 0:96]), reads=["tmpf1", "sm0"], writes=["sm0"])
        P.op("dve", TT(t[:, 0, 2:3], t[:, 0, 0:1], t[:, 0, 1:2], ALU.mult), reads=["sm0"], writes=["sm0"])
        P.op("act", ACTV(t[:, 0, 3:4], t[:, 0, 2:3], AF.Sqrt), reads=["sm0"], writes=["sm0"])
        P.op("dve", TS1(self.negC[:], t[:, 0, 3:4], -math.sqrt(96.0), ALU.mult), reads=["sm0"], writes=["negC"])

    def load_x(self, bi):
        P = self.P
        for j in range(4):
            tok = slice(j * 512, (j + 1) * 512)
            P.op("sp", DMA(self.xT[:, :, tok], self.d["xT"][bi, :, tok].rearrange("(c p) t -> p c t", p=128)),
                 writes=[("xT", c, j) for c in range(8)], dma=True, semq="bg")

    def store_x(self, bi):
        P = self.P
        for j in range(4):
            tok = slice(j * 512, (j + 1) * 512)
            P.op("sp", DMA(self.outT[bi, :, tok].rearrange("(c p) t -> p c t", p=128), self.xT[:, :, tok]),
                 reads=[("xT", c, j) for c in range(8)], dma=True, semq="bg")

    def rmsnorm_T(self, srcs, ntok, neps, scales, shifts, dsts, psb, extra=None):
        P = self.P
        n = len(srcs)
        ps = self.ps[psb]
        for c, (src, skey) in enumerate(srcs):
            r = self.rot("sq", 2)
            P.op("act", ACTV(self.sq[r][:, 0:ntok], src, AF.Square), reads=[skey], writes=[("sq", r)])
            P.op("pe", MM(ps[:, 0:ntok], self.ones_b[:], self.sq[r][:, 0:ntok], c == 0, c == n - 1),
                 reads=[("sq", r), "ones_b"], writes=[("ps", psb)])
        P.op("act", ACTV(self.rstdB[:, 0:ntok], ps[:, 0:ntok], AF.Sqrt, bias=self.cst[:, neps:neps + 1]),
             reads=[("ps", psb), "cst"], writes=["rstdB"])
        P.op("dve", lambda e: e.reciprocal(out=self.rstdB[:, 0:ntok], in_=self.rstdB[:, 0:ntok]),
             reads=["rstdB"], writes=["rstdB"])
        for c, (src, skey) in enumerate(srcs):
            dst, dkey = dsts[c]
            if shifts is None:
                P.op("dve", STT(dst, src, scales[c], self.rstdB[:, 0:ntok], ALU.mult, ALU.mult),
                     reads=[skey, "rstdB"], writes=[dkey])
            else:
                r = self.rot("tmpf", 2)
                P.op("dve", STT(self.tmpf[r][:, 0:ntok], src, scales[c], self.rstdB[:, 0:ntok], ALU.mult, ALU.mult),
                     reads=[skey, "rstdB"], writes=[("tmpf", r)])
                P.op("act", ACTV(dst, self.tmpf[r][:, 0:ntok], AF.Identity, bias=shifts[c]),
                     reads=[("tmpf", r)], writes=[dkey])
                if extra is not None:
                    extra(c, dst, dkey)

    def phase_moe(self, l, bi):
        P, d = self.P, self.d
        G = 2
        h2T = self.av(0, [8, S])
        wgu = [self.av(16384 + s * 4096, [2, 8, 256]) for s in range(3)]
        wdn = [self.av(28672 + s * 2048, [2, 1024]) for s in range(4)]
        actT = [self.av(36864 + g * 4096, [2, S]) for g in range(G)]
        h2f = self.av(36864, [8, 512], F32)
        dwT = self.av(45056, [S], parts=32)
        sm = self.sm
        g2 = lambda m: self.modT[:, l, 40 + m, bi:bi + 1]
        for j in range(4):
            tok = slice(j * 512, (j + 1) * 512)
            srcs = [(self.xT[:, c, tok], ("xT", c, j)) for c in range(8)]
            dsts = [(h2f[:, c, :], ("h2f", c)) for c in range(8)]
            scales = [self.scl2[:, l, c, bi:bi + 1] for c in range(8)]
            shifts = [self.modT[:, l, 24 + c, bi:bi + 1] for c in range(8)]

            def extra(c, dst, dkey, j=j, tok=tok):
                P.op("pool", CP(h2T[:, c, tok], dst), reads=[dkey], writes=[("h2T", c, j)])
            self.rmsnorm_T(srcs, 512, 0, scales, shifts, dsts, 7, extra)
            for t in range(4):
                tt = slice(t * 128, (t + 1) * 128)
                pb = 5 + self.rot("rt_ps", 2)
                psl = self.ps[pb]
                P.op("pe", MM(psl[:, 0:36], self.ones_f[0:1, :], self.rows_f[0:1, l * 36:(l + 1) * 36], True, False),
                     reads=["ones_f", "rows_f"], writes=[("ps", pb)])
                for kc in range(8):
                    P.op("pe", MM(psl[:, 0:36], h2f[:, kc, tt], self.wr[:, l, kc, :], False, kc == 7),
                         reads=[("h2f", kc), "wr"], writes=[("ps", pb)])
                k = "rt"
                lg = sm[:, 0, :]
                P.op("act", ACTV(lg, psl[:, 0:36], AF.Copy), reads=[("ps", pb)], writes=[k])
                gmax, ngmax, gs, gw = sm[:, 1, 0:1], sm[:, 1, 1:2], sm[:, 1, 2:3], sm[:, 1, 3:4]
                m1, m2, dn, w1, w2 = sm[:, 1, 4:5], sm[:, 1, 5:6], sm[:, 1, 6:7], sm[:, 1, 7:8], sm[:, 1, 8:9]
                ohg, pen, eg = sm[:, 2, 0:4], sm[:, 2, 4:8], sm[:, 2, 8:12]
                elm, oh1, elm2, oh2, dw = sm[:, 3, 0:32], sm[:, 4, 0:32], sm[:, 5, 0:32], sm[:, 6, 0:32], sm[:, 7, 0:32]
                o = lambda eng, fn: P.op(eng, fn, reads=[k], writes=[k])
                o("dve", RMAX(gmax, lg[:, 0:4]))
                o("dve", TS1(ohg, lg[:, 0:4], gmax, ALU.is_equal))
                o("dve", TS1(ngmax, gmax, -1.0, ALU.mult))
                o("act", ACTV(eg, lg[:, 0:4], AF.Exp, bias=ngmax))
                o("dve", RSUM(gs, eg))
                o("dve", lambda e: e.reciprocal(out=gw, in_=gs))
                o("dve", TS(pen, ohg, 1.0, 1.0e4, ALU.subtract, ALU.mult))
                o("dve", TT(elm.rearrange("p (a b) -> p a b", a=4), lg[:, 4:36].rearrange("p (a b) -> p a b", a=4),
                            pen.unsqueeze(2).broadcast_to([128, 4, 8]), ALU.add))
                o("dve", RMAX(m1, elm))
                o("dve", TS1(oh1, elm, m1, ALU.is_equal))
                o("dve", STT(elm2, oh1, -1.0e4, elm, ALU.mult, ALU.add))
                o("dve", RMAX(m2, elm2))
                o("dve", TS1(oh2, elm2, m2, ALU.is_equal))
                o("dve", TT(dn, m2, m1, ALU.subtract))
                o("act", ACTV(w2, dn, AF.Sigmoid))
                o("act", ACTV(w1, dn, AF.Sigmoid, scale=-1.0))
                o("dve", TT(w1, w1, gw, ALU.mult))
                o("dve", TT(w2, w2, gw, ALU.mult))
                o("dve", TS1(dw, oh1, w1, ALU.mult))
                o("dve", STT(dw, oh2, w2, dw, ALU.mult, ALU.add))
                pt = self.ps[pb]
                P.op("pe", TR(pt[0:32, 128:256], dw, self.ident_f[:]), reads=[k, "ident_f"], writes=[("ps", pb)])
                P.op("act", ACTV(dwT[0:32, j * 512 + t * 128:j * 512 + (t + 1) * 128], pt[0:32, 128:256], AF.Copy),
                     reads=[("ps", pb)], writes=[("dwT", j)])
        P.barrier()
        for e in range(32):
            s3 = e % 3
            s4 = e % 4
            wg_, wu_ = wgu[s3][:, 0], wgu[s3][:, 1]
            P.op("pool", DMA(wg_, d["moe_w_gate"][l, e].rearrange("(kc p) n -> p kc n", p=128)),
                 writes=[("wg", s3)], dma=True)
            P.op("pool", DMA(wu_, d["moe_w_up"][l, e].rearrange("(kc p) n -> p kc n", p=128)),
                 writes=[("wu", s3)], dma=True)
            P.op("pool", DMA(wdn[s4], d["moe_w_down"][l, e].rearrange("(kc p) n -> p kc n", p=128)),
                 writes=[("wd", s4)], dma=True)
            ge = e % G
            for j in range(4):
                tok = slice(j * 512, (j + 1) * 512)
                pw = 4 + self.rot("pw", 2)
                P.op("pe", MM(self.ps[pw][:], self.sel[0:32, e, :], dwT[0:32, tok], True, True),
                     reads=["sel", ("dwT", j)], writes=[("ps", pw)])
                for c in range(2):
                    r = self.rot("gu", 2)
                    pg, pu = 0 + r, 2 + r
                    for kc in range(8):
                        P.op("pe", MM(self.ps[pg][:], wg_[:, kc, c * 128:(c + 1) * 128], h2T[:, kc, tok], kc == 0, kc == 7),
                             reads=[("wg", s3), ("h2T", kc, j)], writes=[("ps", pg)])
                    for kc in range(8):
                        P.op("pe", MM(self.ps[pu][:], wu_[:, kc, c * 128:(c + 1) * 128], h2T[:, kc, tok], kc == 0, kc == 7),
                             reads=[("wu", s3), ("h2T", kc, j)], writes=[("ps", pu)])
                    rb = self.rot("tmpb", 2)
                    sg, tb = self.tmpb[rb], self.tmpb[2 + rb]
                    P.op("act", ACTV(sg[:], self.ps[pg][:], AF.Silu), reads=[("ps", pg)], writes=[("tmpb", rb)])
                    P.op("dve", TT(tb[:], sg[:], self.ps[pu][:], ALU.mult), reads=[("tmpb", rb), ("ps", pu)],
                         writes=[("tmpb", 2 + rb)])
                    P.op("dve", TT(actT[ge][:, c, tok], tb[:], self.ps[pw][:], ALU.mult),
                         reads=[("tmpb", 2 + rb), ("ps", pw)], writes=[("actT", ge, c, j)])
            if ge == G - 1:
                for j in range(4):
                    tok = slice(j * 512, (j + 1) * 512)
                    for m in range(8):
                        py = 6 + self.rot("py", 2)
                        n = 0
                        for gg in range(G):
                            ee = e - (G - 1) + gg
                            for kc in range(2):
                                P.op("pe", MM(self.ps[py][:], wdn[ee % 4][:, kc, m * 128:(m + 1) * 128], actT[gg][:, kc, tok],
                                              n == 0, n == 2 * G - 1),
                                     reads=[("wd", ee % 4), ("actT", gg, kc, j)], writes=[("ps", py)])
                                n += 1
                        P.op("dve", STT(self.xT[:, m, tok], self.ps[py][:], g2(m), self.xT[:, m, tok], ALU.mult, ALU.add),
                             reads=[("ps", py), ("xT", m, j), "modT"], writes=[("xT", m, j)])

    def phase_smoe(self, l, bi):
        P, d = self.P, self.d
        I32 = mybir.dt.int32
        sm = self.sm
        av = self.av
        g2 = lambda m: self.modT[:, l, 40 + m, bi:bi + 1]
        iota_r, iota_p = self.iota[:, 0:64], self.iota[:, 64:65]
        NT = 63
        h2f_ = [av(0, [8, 512], F32), av(25600, [8, 512], F32)]
        h2b_ = [av(8192, [8, 512]), av(33792, [8, 512])]
        hrow = [av(12288 + i * 1024, [1024]) for i in range(2)]
        oh_all = av(14336, [16, 2, 32], F32)
        pre_all = av(16384, [16, 32], F32)
        pos_all = av(17408, [16, 32], F32)
        wts_all = av(18432, [16, 2], F32)
        vals_all = av(18496, [16, 2, 4], F32)
        slot_f = av(18752, [16, 2], F32)
        slot_i = av(18816, [16, 2], I32 if False else F32).bitcast(I32)
        cmp = av(18880, [64, 32], F32)
        te = av(22976, [64], F32)
        ohcum = av(23232, [32])
        base_r = av(23392, [32], F32)
        nt_r = av(23456, [32], F32)
        end_r = av(23520, [32], F32)
        ntB = av(23584, [128], parts=32)
        tmp32 = av(23712, [16, 32], F32)
        sldef = av(24736, [64, 4], F32)
        widx = av(52800, [64], F32).bitcast(I32)
        self.issue_casts(None, layer=l)
        wkeys = [("wcast", l, q) for q in range(12)]
        if not hasattr(self, "_bc"):
            self._bc = {}

            def mk(e):
                self._bc["r"] = e.alloc_register("bcreg")
                return e.reg_mov(self._bc["r"], 4095)
            P.op("pool", mk)
        bc = self._bc
        P.op("dve", MSET(sldef, 0.0), writes=["sldef"])
        P.op("dve", MSET(sldef[:, :, 1:2], 6000.0), writes=["sldef"])
        P.op("sp", DMA(self.SL.rearrange("(p s) c -> p s c", s=64), sldef), reads=["sldef"], writes=["SL"], dma=True)
        ohb4 = av(23264, [4, 32])
        pbs = {}

        def frontA(j):
            tok = slice(j * 512, (j + 1) * 512)
            T0 = j * 4
            h2f, h2b = h2f_[j % 2], h2b_[j % 2]
            hk = j % 2
            srcs = [(self.xT[:, c, tok], ("xT", c, j)) for c in range(8)]
            dsts = [(h2f[:, c, :], ("h2f", hk, c)) for c in range(8)]
            scales = [self.scl2[:, l, c, bi:bi + 1] for c in range(8)]
            shifts = [self.modT[:, l, 24 + c, bi:bi + 1] for c in range(8)]

            def extra(c, dst, dkey, h2b=h2b, hk=hk):
                P.op("pool", CP(h2b[:, c, :], dst), reads=[dkey], writes=[("h2b", hk, c)])
            self.rmsnorm_T(srcs, 512, 0, scales, shifts, dsts, 7, extra)

        def frontB(j):
            T0 = j * 4
            h2f, h2b = h2f_[j % 2], h2b_[j % 2]
            hk = j % 2
            pb = 5 + self.rot("rt_ps", 2)
            psl = self.ps[pb]
            for t in range(4):
                T = T0 + t
                tt = slice(t * 128, (t + 1) * 128)
                p4 = 3 + self.rot("hrps", 2)
                ptb = self.ps[p4][:].bitcast(BF16)
                for kc in range(8):
                    P.op("pe", TR(ptb[:, kc * 128:(kc + 1) * 128], h2b[:, kc, tt], self.ident_b[:]),
                         reads=[("h2b", hk, kc), "ident_b"], writes=[("ps", p4)])
                hr = self.rot("hrow", 2)
                P.op("act", ACTV(hrow[hr], ptb[:, 0:1024], AF.Copy), reads=[("ps", p4)], writes=[("hrow", hr)])
                P.op("sp", DMA(self.hs[T * 128:(T + 1) * 128, :], hrow[hr]), reads=[("hrow", hr)], writes=[("hs", T)], dma=True)
                P.op("pe", MM(psl[:, t * 36:(t + 1) * 36], self.ones_f[0:1, :], self.rows_f[0:1, l * 36:(l + 1) * 36], True, False),
                     reads=["ones_f", "rows_f"], writes=[("ps", pb)])
                for kc in range(8):
                    P.op("pe", MM(psl[:, t * 36:(t + 1) * 36], h2f[:, kc, tt], self.wr[:, l, kc, :], False, kc == 7),
                         reads=[("h2f", hk, kc), "wr"], writes=[("ps", pb)])
            pbs[j] = pb

        def chain(j):
            T0 = j * 4
            pb = pbs[j]
            psl = self.ps[pb]
            k = "rt"
            lg = sm[:, 0:4, :]
            P.op("act", ACTV(lg, psl[:, 0:144].rearrange("p (t e) -> p t e", t=4), AF.Copy), reads=[("ps", pb)], writes=[k])
            gmax, gs, gw, m1 = sm[:, 4, 0:4], sm[:, 4, 4:8], sm[:, 4, 8:12], sm[:, 4, 12:16]
            m2, dn, w1, w2 = sm[:, 4, 16:20], sm[:, 4, 20:24], sm[:, 4, 24:28], sm[:, 4, 28:32]
            v3 = lambda ap: ap.rearrange("p (t g) -> p t g", t=4)
            ohg, pen, eg = v3(sm[:, 5, 0:16]), v3(sm[:, 5, 16:32]), v3(sm[:, 6, 0:16])
            elm, elm2 = sm[:, 7:11, 0:32], sm[:, 11:15, 0:32]
            oh1, oh2 = oh_all[:, T0:T0 + 4, 0, :], oh_all[:, T0:T0 + 4, 1, :]
            b4 = lambda ap, n: ap.unsqueeze(2).broadcast_to([128, 4, n])
            o = lambda eng, fn: P.op(eng, fn, reads=[k], writes=[k])
            o("dve", RMAX(gmax, lg[:, :, 0:4]))
            o("dve", TT(ohg, lg[:, :, 0:4], b4(gmax, 4), ALU.is_equal))
            o("dve", TT(eg, lg[:, :, 0:4], b4(gmax, 4), ALU.subtract))
            o("act", ACTV(eg, eg, AF.Exp))
            o("dve", RSUM(gs, eg))
            o("dve", lambda e: e.reciprocal(out=gw, in_=gs))
            o("dve", TS(pen, ohg, 1.0, 1.0e4, ALU.subtract, ALU.mult))
            o("dve", TT(elm.rearrange("p t (g e) -> p t g e", g=4), lg[:, :, 4:36].rearrange("p t (g e) -> p t g e", g=4),
                        pen.unsqueeze(3).broadcast_to([128, 4, 4, 8]), ALU.add))
            o("dve", RMAX(m1, elm))
            o("dve", TT(oh1, elm, b4(m1, 32), ALU.is_equal))
            o("dve", STT(elm2, oh1, -1.0e4, elm, ALU.mult, ALU.add))
            o("dve", RMAX(m2, elm2))
            o("dve", TT(oh2, elm2, b4(m2, 32), ALU.is_equal))
            o("dve", TT(dn, m2, m1, ALU.subtract))
            o("act", ACTV(w2, dn, AF.Sigmoid))
            o("act", ACTV(w1, dn, AF.Sigmoid, scale=-1.0))
            o("dve", TT(wts_all[:, T0:T0 + 4, 0], w1, gw, ALU.mult))
            o("dve", TT(wts_all[:, T0:T0 + 4, 1], w2, gw, ALU.mult))
            P.op("dve", TT(ohb4, oh1, oh2, ALU.add), reads=[k], writes=["ohb4"])
            for t in range(4):
                T = T0 + t
                outp = psl[:, 160 + t * 32:160 + (t + 1) * 32]
                last = (T == 0)
                P.op("pe", MM(outp, self.U_b[:], ohb4[:, t, :], True, last), reads=["U_b", "ohb4"], writes=[("ps", pb)])
                if j > 0:
                    P.op("pe", MM(outp, self.ones_b[:], ohcum, False, t == 0), reads=["ones_b", "ohcum"], writes=[("ps", pb)])
                for t2 in range(t):
                    P.op("pe", MM(outp, self.ones_b[:], ohb4[:, t2, :], False, t2 == t - 1), reads=["ones_b", "ohb4"], writes=[("ps", pb)])
            P.op("act", ACTV(pre_all[:, T0:T0 + 4, :], psl[:, 160:288].rearrange("p (t e) -> p t e", t=4), AF.Copy),
                 reads=[("ps", pb)], writes=[("pre", j)])
            bsum = sm[:, 15, 0:32]
            P.op("dve", RSUM(bsum, ohb4.rearrange("p t e -> p e t")), reads=["ohb4"], writes=["bsum"])
            if j == 0:
                P.op("dve", CP(ohcum, bsum), reads=["bsum"], writes=["ohcum"])
            else:
                P.op("dve", TT(ohcum, ohcum, bsum, ALU.add), reads=["bsum", "ohcum"], writes=["ohcum"])

        frontA(0)
        frontA(1)
        frontB(0)
        frontA(2)
        frontB(1)
        chain(0)
        frontA(3)
        frontB(2)
        chain(1)
        frontB(3)
        chain(2)
        chain(3)
        k = "rt"
        pn = self.ps[5]
        ntc, vv, fr = sm[0:32, 18, 0:1], sm[0:32, 18, 1:2], sm[0:32, 18, 2:3]
        P.op("pe", MM(pn[0:32, 0:1], ohcum, self.ones_b[:, 0:1], True, True), reads=["ohcum", "ones_b"], writes=[("ps", 5)])
        thr = sm[0:32, 16, 0:16]
        cmt = sm[0:32, 17, 0:16]
        P.op("dve", CP(vv, pn[0:32, 0:1]), reads=[("ps", 5)], writes=[k])
        P.op("dve", TS1(thr, iota_r[0:32, 0:16], 128.0, ALU.mult), reads=["iota"], writes=[k])
        P.op("dve", TS1(cmt, thr, vv, ALU.is_lt), reads=[k], writes=[k])
        P.op("dve", RSUM(ntc, cmt), reads=[k], writes=[k])
        P.op("dve", CP(ntB, ntc.broadcast_to([32, 128])), reads=[k], writes=["ntB"])
        pbb = self.ps[6]
        P.op("pe", MM(pbb[:, 0:32], ntB, self.U_b[0:32, 0:32], True, True), reads=["ntB", "U_b"], writes=[("ps", 6)])
        P.op("pe", MM(pbb[:, 32:64], ntB, self.ident_b[0:32, 0:32], True, True), reads=["ntB", "ident_b"], writes=[("ps", 6)])
        P.op("act", ACTV(base_r, pbb[:, 0:32], AF.Copy), reads=[("ps", 6)], writes=["base_r"])
        P.op("act", ACTV(nt_r, pbb[:, 32:64], AF.Copy), reads=[("ps", 6)], writes=["nt_r"])
        P.op("dve", TT(end_r, base_r, nt_r, ALU.add), reads=["base_r", "nt_r"], writes=["end_r"])
        P.op("dve", STT(pos_all, base_r.unsqueeze(1).broadcast_to([128, 16, 32]), 128.0, pre_all, ALU.mult, ALU.add),
             reads=["base_r"] + [("pre", jj) for jj in range(4)], writes=["pos_all"])
        for r in range(2):
            P.op("dve", TT(tmp32, oh_all[:, :, r, :], pos_all, ALU.mult), reads=["rt", "pos_all"], writes=["tmp32"])
            P.op("dve", RSUM(slot_f[:, :, r], tmp32), reads=["tmp32"], writes=["slot_f"])
            P.op("dve", TS(vals_all[:, :, r, 0], iota_r[:, 0:16], 128.0, iota_p, ALU.mult, ALU.add), reads=["iota"], writes=["vals"])
            P.op("dve", TS(vals_all[:, :, r, 1], vals_all[:, :, r, 0], 1.0, float(r * S), ALU.mult, ALU.add), reads=["vals"], writes=["vals"])
            P.op("dve", CP(vals_all[:, :, r, 2], wts_all[:, :, r]), reads=["rt"], writes=["vals"])
            P.op("dve", MSET(vals_all[:, :, r, 3], 0.0), writes=["vals"])
        P.op("dve", CP(slot_i, slot_f), reads=["slot_f"], writes=["slot_i"])
        for T in range(16):
            for r in range(2):
                P.op("pool", (lambda e, T=T, r=r: e.indirect_dma_start(
                    out=self.SL, out_offset=bass.IndirectOffsetOnAxis(ap=slot_i[:, T, r:r + 1], axis=0),
                    in_=vals_all[:, T, r, :], in_offset=None)), reads=["slot_i", "vals", "SL"], writes=[("SLw", T, r)], dma=True)
        P.op("dve", TT(cmp, end_r.unsqueeze(1).broadcast_to([128, 64, 32]), iota_r.unsqueeze(2).broadcast_to([128, 64, 32]), ALU.is_le),
             reads=["end_r", "iota"], writes=["cmp"])
        P.op("dve", RSUM(te, cmp), reads=["cmp"], writes=["te"])
        P.op("dve", TS(te, te, 128.0, iota_p, ALU.mult, ALU.add), reads=["te", "iota"], writes=["te"])
        P.op("dve", CP(widx, te), reads=["te"], writes=["widx"])
        P.barrier()
        NBUF = 6
        wgu_sb = [av(i * 4096, [8, 512]) for i in range(NBUF)]
        wd_sb = [av(24576 + i * 2048, [2, 1024]) for i in range(NBUF)]
        hg = [av(36864 + i * 1024, [1024]) for i in range(NBUF)]
        hgT = [av(43008 + i * 1024, [8, 128]) for i in range(2)]
        a_sb = [av(45056 + i * 256, [256]) for i in range(2)]
        aT = [av(45568 + i * 256, [2, 128]) for i in range(2)]
        osb = [av(46080 + i * 2048, [1024], F32) for i in range(2)]
        SLall = av(50176, [64, 4], F32)
        sidx_all = av(50688, [64, 2], F32).bitcast(I32)
        P.op("sp", DMA(SLall, self.SL.rearrange("(j p) c -> p j c", p=128)), writes=["SLall"], dma=True)
        P.op("dve", CP(sidx_all, SLall[:, :, 0:2]), reads=["SLall"], writes=["sidx_all"])

        def loads(jt):
            r3 = jt % NBUF
            P.op("pool", (lambda e: e.indirect_dma_start(
                out=wgu_sb[r3].rearrange("p a b -> p (a b)"), out_offset=None, in_=self.wgu_b[l],
                in_offset=bass.IndirectOffsetOnAxis(ap=widx[:, jt:jt + 1], axis=0), bounds_check=bc["r"], oob_is_err=False)),
                reads=["widx"] + wkeys, writes=[("wgu", r3)], dma=True)
            P.op("pool", (lambda e: e.indirect_dma_start(
                out=wd_sb[r3].rearrange("p a b -> p (a b)"), out_offset=None, in_=self.wd_b[l],
                in_offset=bass.IndirectOffsetOnAxis(ap=widx[:, jt:jt + 1], axis=0), bounds_check=bc["r"], oob_is_err=False)),
                reads=["widx"] + wkeys, writes=[("wd", r3)], dma=True)
            P.op("pool", (lambda e: e.indirect_dma_start(
                out=hg[r3], out_offset=None, in_=self.hs, in_offset=bass.IndirectOffsetOnAxis(ap=sidx_all[:, jt, 0:1], axis=0))),
                reads=["sidx_all"], writes=[("hg", r3)], dma=True)

        for q in range(NBUF):
            loads(q)

        def S1(jt):
            r2, r3 = jt % 2, jt % NBUF
            pt = 0 + r2
            ptb = self.ps[pt][:].bitcast(BF16)
            for kc in range(8):
                P.op("pe", TR(ptb[:, kc * 128:(kc + 1) * 128], hg[r3][:, kc * 128:(kc + 1) * 128], self.ident_b[:]),
                     reads=[("hg", r3), "ident_b"], writes=[("ps", pt)])
            P.op("act", ACTV(hgT[r2], ptb[:, 0:1024].rearrange("p (a b) -> p a b", a=8), AF.Copy),
                 reads=[("ps", pt)], writes=[("hgT", r2)])

        def S2(jt):
            r2, r3 = jt % 2, jt % NBUF
            pg = 2 + r2
            for kc in range(8):
                P.op("pe", MM(self.ps[pg][:], hgT[r2][:, kc, :], wgu_sb[r3][:, kc, :], kc == 0, kc == 7),
                     reads=[("hgT", r2), ("wgu", r3)], writes=[("ps", pg)])
            sg = self.tmpf[r2]
            P.op("act", ACTV(sg[:, 0:256], self.ps[pg][:, 0:256], AF.Silu), reads=[("ps", pg)], writes=[("tmpf", r2)])
            P.op("dve", STT(a_sb[r2], sg[:, 0:256], SLall[:, jt, 2:3], self.ps[pg][:, 256:512], ALU.mult, ALU.mult),
                 reads=[("tmpf", r2), "SLall", ("ps", pg)], writes=[("a", r2)])

        def S3(jt):
            r2 = jt % 2
            pt = 6 + r2
            ptb = self.ps[pt][:].bitcast(BF16)
            for c in range(2):
                P.op("pe", TR(ptb[:, c * 128:(c + 1) * 128], a_sb[r2][:, c * 128:(c + 1) * 128], self.ident_b[:]),
                     reads=[("a", r2), "ident_b"], writes=[("ps", pt)])
            P.op("act", ACTV(aT[r2], ptb[:, 0:256].rearrange("p (a b) -> p a b", a=2), AF.Copy),
                 reads=[("ps", pt)], writes=[("aT", r2)])

        def S4(jt):
            r2, r3 = jt % 2, jt % NBUF
            for hf in range(2):
                po = 4 + hf
                for kc in range(2):
                    P.op("pe", MM(self.ps[po][:], aT[r2][:, kc, :], wd_sb[r3][:, kc, hf * 512:(hf + 1) * 512], kc == 0, kc == 1),
                         reads=[("aT", r2), ("wd", r3)], writes=[("ps", po)])
                P.op("act" if hf == 0 else "dve", (ACTV(osb[r2][:, 0:512], self.ps[po][:], AF.Copy) if hf == 0 else
                                                   CP(osb[r2][:, 512:1024], self.ps[po][:])),
                     reads=[("ps", po)], writes=[("osb", r2, hf)])
            P.op("pool", (lambda e: e.indirect_dma_start(
                out=self.Y, out_offset=bass.IndirectOffsetOnAxis(ap=sidx_all[:, jt, 1:2], axis=0), in_=osb[r2], in_offset=None,
                bounds_check=bc["r"], oob_is_err=False)),
                reads=["sidx_all", ("osb", r2, 0), ("osb", r2, 1)], writes=[("Y", jt)], dma=True)
            if jt + NBUF < NT + 0 and jt + NBUF - 1 < NT:
                pass

        for k in range(NT + 3):
            if k < NT:
                S1(k)
            if 0 <= k - 1 < NT:
                S2(k - 1)
            if 0 <= k - 2 < NT:
                S3(k - 2)
            if 0 <= k - 3 < NT:
                S4(k - 3)
                if (k - 3) + NBUF < NT:
                    loads((k - 3) + NBUF)
        P.barrier()
        ysum = [av(i * 2048, [1024], F32) for i in range(4)]
        ytmp = [av(8192 + i * 2048, [1024], F32) for i in range(2)]
        for j in range(4):
            for t in range(4):
                T = j * 4 + t
                yt = self.rot("ytmp", 2)
                P.op("sp", DMA(ysum[t], self.Y[T * 128:(T + 1) * 128, :]), writes=[("ysum", t)], dma=True)
                P.op("sp", DMA(ytmp[yt], self.Y[S + T * 128:S + (T + 1) * 128, :]), writes=[("ytmp", yt)], dma=True)
                P.op("dve", TT(ysum[t], ysum[t], ytmp[yt], ALU.add), reads=[("ysum", t), ("ytmp", yt)], writes=[("ysum", t)])
            for m in range(8):
                pc = self.rot("cps", 4)
                for t in range(4):
                    P.op("pe", TR(self.ps[pc][:, t * 128:(t + 1) * 128], ysum[t][:, m * 128:(m + 1) * 128], self.ident_f[:]),
                         reads=[("ysum", t), "ident_f"], writes=[("ps", pc)])
                tok = slice(j * 512, (j + 1) * 512)
                P.op("dve", STT(self.xT[:, m, tok], self.ps[pc][:], g2(m), self.xT[:, m, tok], ALU.mult, ALU.add),
                     reads=[("ps", pc), ("xT", m, j), "modT"], writes=[("xT", m, j)])

    def phase_mix1(self, bi):
        P, d = self.P, self.d
        l = 1
        hT = self.av(0, [8, 512])
        wsl = [self.av(4096 + s * 4096, [8, 512]) for s in range(3)]
        wos = [self.av(16384 + s * 4096, [16, 256]) for s in range(2)]
        vn = self.av(24576, [4, 2048])
        gT = self.av(32768, [16, 512])
        lngB = self.av(40960, [1, 2048])[:, 0]
        lnbB = self.av(43008, [1, 2048])[:, 0]
        wsT = self.av(45056, [8, 128])
        rowsb = self.av(46080, [1, 3072], parts=1)[:, 0]
        P.op("pool", DMA(rowsb, d["rows"][:, R_CBV:R_CBV + 3072]), writes=["rows_b"], dma=True)
        sm = self.sm
        g1 = lambda m: self.modT[:, l, 16 + m, bi:bi + 1]
        if self.sparse:
            while self.cast_jobs and self.cast_jobs[0][3][0] == "cwcast":
                self.issue_casts(1)
            cw_in, cw_out, cwk = self.cwin_b, self.cwout_b, [("cwcast", q) for q in range(4)]
        else:
            cw_in, cw_out, cwk = d["c_w_in"], d["c_w_out"], []
        P.op("pool", DMA(lngB, d["c_ln_g"][0].partition_broadcast(128)), writes=["lngB"], dma=True)
        P.op("pool", DMA(lnbB, d["c_ln_b"][0].partition_broadcast(128)), writes=["lnbB"], dma=True)
        P.op("pool", DMA(wsT, d["w_sT"]), writes=["wsT"], dma=True)
        for j in range(4):
            tok = slice(j * 512, (j + 1) * 512)
            srcs = [(self.xT[:, c, tok], ("xT", c, j)) for c in range(8)]
            dsts = [(hT[:, c, :], ("hT", c)) for c in range(8)]
            scales = [self.scl1[:, l, c, bi:bi + 1] for c in range(8)]
            shifts = [self.modT[:, l, 0 + c, bi:bi + 1] for c in range(8)]
            self.rmsnorm_T(srcs, 512, 0, scales, shifts, dsts, 7)
            for s in range(4):
                slot = self.rot("wsl", 3)
                w = wsl[slot]
                P.op("pool", DMA(w, cw_in[:, 2048 + s * 512:2048 + (s + 1) * 512].rearrange("(kc p) n -> p kc n", p=128)),
                     reads=cwk, writes=[("wsl", slot)], dma=True)
                for t in range(4):
                    pb = self.rot("m1ps", 4)
                    ps = self.ps[pb]
                    P.op("pe", MM(ps[:], self.ones_b[0:1, :], rowsb[0:1, s * 512:(s + 1) * 512], True, False),
                         reads=["ones_b", "rows_b"], writes=[("ps", pb)])
                    for kc in range(8):
                        P.op("pe", MM(ps[:], hT[:, kc, t * 128:(t + 1) * 128], w[:, kc, :], False, kc == 7),
                             reads=[("hT", kc), ("wsl", slot)], writes=[("ps", pb)])
                    P.op("act", ACTV(vn[:, t, s * 512:(s + 1) * 512], ps[:], AF.Gelu_apprx_tanh),
                         reads=[("ps", pb)], writes=[("vn", t, s)])
                    P.op("dve", lambda e, t=t, s=s: e.bn_stats(out=sm[:, 8 + t, s * 6:(s + 1) * 6], in_=vn[:, t, s * 512:(s + 1) * 512]),
                         reads=[("vn", t, s)], writes=[("bst", t)])
            for t in range(4):
                mv = sm[:, 12 + t, 0:2]
                rstd, nmr = sm[:, 12 + t, 2:3], sm[:, 12 + t, 3:4]
                vk = [("vn", t, s) for s in range(4)]
                P.op("dve", lambda e, t=t, mv=mv: e.bn_aggr(out=mv, in_=sm[:, 8 + t, 0:24]), reads=[("bst", t)], writes=[("mv", t)])
                P.op("act", ACTV(rstd, mv[:, 1:2], AF.Sqrt, bias=self.cst[:, 4:5]), reads=[("mv", t), "cst"], writes=[("mv", t)])
                P.op("dve", lambda e, rstd=rstd: e.reciprocal(out=rstd, in_=rstd), reads=[("mv", t)], writes=[("mv", t)])
                P.op("dve", STT(nmr, mv[:, 0:1], -1.0, rstd, ALU.mult, ALU.mult), reads=[("mv", t)], writes=[("mv", t)])
                P.op("dve", TS(vn[:, t, :], vn[:, t, :], rstd, nmr, ALU.mult, ALU.add), reads=vk + [("mv", t)], writes=vk)
                P.op("dve", TT(vn[:, t, :], vn[:, t, :], lngB, ALU.mult), reads=vk + ["lngB"], writes=vk)
                P.op("dve", TT(vn[:, t, :], vn[:, t, :], lnbB, ALU.add), reads=vk + ["lnbB"], writes=vk)
            for su in range(4):
                slot = self.rot("wsl", 3)
                w = wsl[slot]
                P.op("pool", DMA(w, cw_in[:, su * 512:(su + 1) * 512].rearrange("(kc p) n -> p kc n", p=128)),
                     reads=cwk, writes=[("wsl", slot)], dma=True)
                for q in range(4):
                    cc = su * 4 + q
                    g = cc // 2
                    pu = self.rot("m1ps", 4)
                    for kc in range(8):
                        P.op("pe", MM(self.ps[pu][:], w[:, kc, q * 128:(q + 1) * 128], hT[:, kc, :], kc == 0, kc == 7),
                             reads=[("wsl", slot), ("hT", kc)], writes=[("ps", pu)])
                    rb = self.rot("tmpb", 4)
                    uT = self.tmpb[rb]
                    P.op("act", ACTV(uT[:], self.ps[pu][:], AF.Gelu_apprx_tanh, bias=self.vecs[:, V_CBU + cc:V_CBU + cc + 1]),
                         reads=[("ps", pu), "vecs"], writes=[("tmpb", rb)])
                    pz = 4 + self.rot("m1pz", 2)
                    pss = self.ps[pz]
                    P.op("pe", MM(pss[:].rearrange("p (a b) -> p a b", a=4), self.ones_b[0:1, :],
                                  rowsb[0:1, 2048 + g * 128:2048 + (g + 1) * 128].unsqueeze(1).broadcast_to([1, 4, 128]),
                                  True, False), reads=["ones_b", "rows_b"], writes=[("ps", pz)])
                    for n in range(4):
                        P.op("pe", MM(pss[:, n * 128:(n + 1) * 128], vn[:, n, cc * 128:(cc + 1) * 128], wsT[:, g, :], False, n == 3),
                             reads=[("vn", n, cc // 4), "wsT"], writes=[("ps", pz)])
                    P.op("dve", TT(gT[:, cc, :], uT[:], pss[:], ALU.mult), reads=[("tmpb", rb), ("ps", pz)], writes=[("gT", cc)])
            for so in range(4):
                slot = self.rot("wos", 2)
                wo = wos[slot]
                P.op("pool", DMA(wo, cw_out[:, so * 256:(so + 1) * 256].rearrange("(kc p) n -> p kc n", p=128)),
                     reads=cwk, writes=[("wos", slot)], dma=True)
                for mm in range(2):
                    m = so * 2 + mm
                    py = 6 + self.rot("py", 2)
                    for kc in range(16):
                        P.op("pe", MM(self.ps[py][:], wo[:, kc, mm * 128:(mm + 1) * 128], gT[:, kc, :], kc == 0, kc == 15),
                             reads=[("wos", slot), ("gT", kc)], writes=[("ps", py)])
                    P.op("dve", STT(self.xT[:, m, tok], self.ps[py][:], g1(m), self.xT[:, m, tok], ALU.mult, ALU.add),
                         reads=[("ps", py), ("xT", m, j), "modT"], writes=[("xT", m, j)])

    def phase_mix0(self, bi):
        P, d = self.P, self.d
        l = 0
        sm = self.sm
        g1 = lambda m: self.modT[:, l, 16 + m, bi:bi + 1]
        qlat = self.av(0, [3, S])
        kvlat = self.av(6144, [2, S + CTX])
        krt = self.av(10752, [18, 32], F32)
        R0 = 11904
        hT = self.av(R0, [8, S + CTX])
        w_in = self.av(30336, [8, 1696])
        zT = self.av(43904, [4, S + 30])
        ctxs = self.av(43904, [8, CTX], F32)
        for (c0, c1) in ((0, 384), (384, 672), (672, 1184), (1184, 1696)):
            P.op("pool", DMA(w_in[:, :, c0:c1], d["a_w_in"][:, c0:c1].rearrange("(kc p) n -> p kc n", p=128)),
                 writes=[("w_in", c0)], dma=True)
        wkey = lambda col: ("w_in", 0 if col < 384 else 384 if col < 672 else 672 if col < 1184 else 1184)
        P.op("sp", DMA(ctxs, d["ctxT"][bi].rearrange("(kc p) n -> p kc n", p=128)), writes=["ctxs"], dma=True)
        srcs = [(ctxs[:, c, :], "ctxs") for c in range(8)]
        dsts = [(hT[:, c, S:S + CTX], ("hT", c, 4)) for c in range(8)]
        self.rmsnorm_T(srcs, CTX, 0, [self.scl1[:, l, c, 2:3] for c in range(8)],
                       [self.modT[:, l, c, 2:3] for c in range(8)], dsts, 7)
        for j in range(4):
            tok = slice(j * 512, (j + 1) * 512)
            srcs = [(self.xT[:, c, tok], ("xT", c, j)) for c in range(8)]
            dsts = [(hT[:, c, tok], ("hT", c, j)) for c in range(8)]
            self.rmsnorm_T(srcs, 512, 0, [self.scl1[:, l, c, bi:bi + 1] for c in range(8)],
                           [self.modT[:, l, c, bi:bi + 1] for c in range(8)], dsts, 7)
        self.issue_casts(10)
        for c in range(4):
            P.op("dve", MSET(zT[:, c, 0:15], 0.0), writes=[("zT", c, "p0")])
            P.op("dve", MSET(zT[:, c, S + 15:S + 30], 0.0), writes=[("zT", c, "p1")])
        for j in range(5):
            ntok = 512 if j < 4 else CTX
            tok = slice(j * 512, j * 512 + ntok)
            hk = lambda kc: ("hT", kc, j)
            if j < 4:
                srcs = []
                for c in range(3):
                    for kc in range(8):
                        P.op("pe", MM(self.ps[c][:, 0:ntok], w_in[:, kc, c * 128:(c + 1) * 128], hT[:, kc, tok], kc == 0, kc == 7),
                             reads=[wkey(c * 128), hk(kc)], writes=[("ps", c)])
                    srcs.append((self.ps[c][:, 0:ntok], ("ps", c)))
                self.rmsnorm_T(srcs, ntok, 1, [self.qkng[:, c:c + 1] for c in range(3)], None,
                               [(qlat[:, c, tok], ("qlat", c, j)) for c in range(3)], 7)
            srcs = []
            for c in range(2):
                for kc in range(8):
                    P.op("pe", MM(self.ps[3 + c][:, 0:ntok], w_in[:, kc, 384 + c * 128:384 + (c + 1) * 128], hT[:, kc, tok], kc == 0, kc == 7),
                         reads=[wkey(384), hk(kc)], writes=[("ps", 3 + c)])
                srcs.append((self.ps[3 + c][:, 0:ntok], ("ps", 3 + c)))
            self.rmsnorm_T(srcs, ntok, 2, [self.qkng[:, 3 + c:4 + c] for c in range(2)], None,
                           [(kvlat[:, c, tok], ("kvlat", c, j)) for c in range(2)], 7)
            PKR, PGL = _Rec(), _Rec()
            for t in range(ntok // 128):
                T = j * 4 + t
                pb = 5 + self.rot("krps", 2)
                for kc in range(8):
                    PKR.op("pe", MM(self.ps[pb][:, 0:32], hT[:, kc, j * 512 + t * 128:j * 512 + (t + 1) * 128], w_in[:, kc, 640:672], kc == 0, kc == 7),
                         reads=[wkey(640), hk(kc)], writes=[("ps", pb)])
                k = "krw"
                raw, sqr, kg = sm[:, 16, 0:32], sm[:, 17, 0:32], sm[:, 18, 0:32]
                t1, t2 = sm[:, 19, 0:16], sm[:, 19, 16:32]
                o = lambda eng, fn: PKR.op(eng, fn, reads=[k, "kgB", "rope"], writes=[k])
                PKR.op("act", ACTV(raw, self.ps[pb][:, 0:32], AF.Copy), reads=[("ps", pb)], writes=[k])
                o("dve", TT(sqr, raw, raw, ALU.mult))
                PKR.op("dve", RSUM(sm[:, 20, T:T + 1], sqr), reads=[k], writes=[k, ("sskr", T)])
                if j < 4:
                    o("dve", TT(kg, raw, self.kgB[:, 64:96], ALU.mult))
                    cs, sn = self.rope[:, T, 0:16], self.rope[:, T, 16:32]
                    o("dve", TT(t1, kg[:, 0:16], cs, ALU.mult))
                    o("dve", TT(t2, kg[:, 16:32], sn, ALU.mult))
                    PKR.op("dve", TT(krt[:, T, 0:16], t1, t2, ALU.subtract), reads=[k], writes=[k, ("krt", T)])
                    o("dve", TT(t1, kg[:, 0:16], sn, ALU.mult))
                    o("dve", TT(t2, kg[:, 16:32], cs, ALU.mult))
                    PKR.op("dve", TT(krt[:, T, 16:32], t1, t2, ALU.add), reads=[k], writes=[k, ("krt", T)])
                else:
                    PKR.op("dve", TT(krt[:, T, :], raw, self.kgB[:, 64:96], ALU.mult), reads=[k, "kgB"], writes=[k, ("krt", T)])
            if j < 4:
                for c in range(4):
                    r = self.rot("glu", 2)
                    pa, pbk = 0 + r, 2 + r
                    for kc in range(8):
                        PGL.op("pe", MM(self.ps[pa][:], w_in[:, kc, 672 + c * 128:672 + (c + 1) * 128], hT[:, kc, tok], kc == 0, kc == 7),
                             reads=[wkey(672), hk(kc)], writes=[("ps", pa)])
                    for kc in range(8):
                        PGL.op("pe", MM(self.ps[pbk][:], w_in[:, kc, 1184 + c * 128:1184 + (c + 1) * 128], hT[:, kc, tok], kc == 0, kc == 7),
                             reads=[wkey(1184), hk(kc)], writes=[("ps", pbk)])
                    rf = self.rot("tmpf", 2)
                    PGL.op("act", ACTV(self.tmpf[rf][:], self.ps[pbk][:], AF.Sigmoid), reads=[("ps", pbk)], writes=[("tmpf", rf)])
                    PGL.op("dve", TT(zT[:, c, 15 + j * 512:15 + (j + 1) * 512], self.ps[pa][:], self.tmpf[rf][:], ALU.mult),
                         reads=[("ps", pa), ("tmpf", rf)], writes=[("zT", c, j)])
            for i_ in range(max(len(PKR.l), len(PGL.l))):
                if i_ < len(PGL.l):
                    P.op(*PGL.l[i_][0], **PGL.l[i_][1])
                if i_ < len(PKR.l):
                    P.op(*PKR.l[i_][0], **PKR.l[i_][1])
        P.barrier()
        Dg = self.av(R0, [4, 31, 128])
        convT = self.av(27776, [4, S])
        w_oc = self.av(35968, [4, D])
        bdw = self.av(40064, [1, 512], parts=1)[:, 0]
        P.op("pool", DMA(bdw, d["rows"][:, R_BDW:R_BDW + 512]), writes=["bdw"], dma=True)
        P.op("pool", DMA(w_oc, d["ab_w_out"][512:1024, :].rearrange("(kc p) n -> p kc n", p=128)), writes=["w_oc"], dma=True)
        self.issue_casts(6)
        for c in range(4):
            P.op("dve", TT(Dg[:, c, :, :], self.ident_b[:].unsqueeze(1).broadcast_to([128, 31, 128]),
                           self.vecs[:, V_DW + c * 31:V_DW + (c + 1) * 31].unsqueeze(2).broadcast_to([128, 31, 128]), ALU.mult),
                 reads=["ident_b", "vecs"], writes=[("Dg", c)])
        zkeys = lambda c: [("zT", c, "p0"), ("zT", c, "p1")] + [("zT", c, jj) for jj in range(4)]
        cst_ = {}

        def convA(t):
            pc = 0 + self.rot("convps", 2)
            ps = self.ps[pc]
            P.op("pe", MM(ps[:], self.ones_b[0:1, :], bdw[0:1, :], True, False), reads=["ones_b", "bdw"], writes=[("ps", pc)])
            for c in range(4):
                for jt in range(31):
                    P.op("pe", MM(ps[:, c * 128:(c + 1) * 128], zT[:, c, t * 128 + jt:t * 128 + jt + 128], Dg[:, c, jt, :],
                                  False, c == 3 and jt == 30), reads=zkeys(c) + [("Dg", c)], writes=[("ps", pc)])
            k = ("cst", t % 2)
            st6, mv = sm[:, 21 + t % 2, 0:6], sm[:, 21 + t % 2, 6:8]
            rstd, nmr = sm[:, 21 + t % 2, 8:9], sm[:, 21 + t % 2, 9:10]
            P.op("dve", lambda e, st6=st6, ps=ps: e.bn_stats(out=st6, in_=ps[:]), reads=[("ps", pc)], writes=[k])
            P.op("dve", lambda e, st6=st6, mv=mv: e.bn_aggr(out=mv, in_=st6), reads=[k], writes=[k])
            P.op("act", ACTV(rstd, mv[:, 1:2], AF.Sqrt, bias=self.cst[:, 4:5]), reads=[k, "cst"], writes=[k])
            P.op("dve", lambda e, rstd=rstd: e.reciprocal(out=rstd, in_=rstd), reads=[k], writes=[k])
            P.op("dve", STT(nmr, mv[:, 0:1], -1.0, rstd, ALU.mult, ALU.mult), reads=[k], writes=[k])
            rb = self.rot("tmpb", 4)
            cn = self.tmpb[rb]
            P.op("act", ACTV(cn[:], ps[:], AF.Identity, bias=nmr, scale=rstd), reads=[k, ("ps", pc)], writes=[("tmpb", rb)])
            cst_[t] = (rb, cn)

        def convB(t):
            rb, cn = cst_[t]
            pt = 2 + self.rot("convpt", 2)
            ptb = self.ps[pt][:].bitcast(BF16)
            for c in range(4):
                P.op("pe", TR(ptb[:, c * 128:(c + 1) * 128], cn[:, c * 128:(c + 1) * 128], self.ident_b[:]),
                     reads=[("tmpb", rb), "ident_b"], writes=[("ps", pt)])
            for c in range(4):
                P.op("act", ACTV(convT[:, c, t * 128:(t + 1) * 128], ptb[:, c * 128:(c + 1) * 128], AF.Silu,
                                 bias=self.vecs[:, V_BLNB + c:V_BLNB + c + 1], scale=self.vecs[:, V_BLNG + c:V_BLNG + c + 1]),
                     reads=[("ps", pt), "vecs"], writes=[("convT", c, t // 4)])

        convA(0)
        for t in range(1, 16):
            convA(t)
            convB(t - 1)
        convB(15)
        for j in range(4):
            tok = slice(j * 512, (j + 1) * 512)
            for m in range(8):
                py = 4 + self.rot("py0", 2)
                for kc in range(4):
                    P.op("pe", MM(self.ps[py][:], w_oc[:, kc, m * 128:(m + 1) * 128], convT[:, kc, tok], kc == 0, kc == 3),
                         reads=["w_oc", ("convT", kc, j)], writes=[("ps", py)])
                P.op("dve", STT(self.xT[:, m, tok], self.ps[py][:], g1(m), self.xT[:, m, tok], ALU.mult, ALU.add),
                     reads=[("ps", py), ("xT", m, j), "modT"], writes=[("xT", m, j)])
        P.barrier()
        QT = self.av(R0, [4, S], parts=96)
        KT = self.av(20096, [4, S + CTX], parts=96)
        Vaug = self.av(29312, [18, 4, 128])
        attnT = self.av(38528, [4, S])
        w_uq = self.av(46720, [3, 384])
        w_ukv = self.av(47872, [2, 512])
        w_oa = self.av(48896, [4, D])
        P.op("pool", DMA(w_oa, d["ab_w_out"][0:512, :].rearrange("(kc p) n -> p kc n", p=128)), writes=["w_oa"], dma=True)
        for hh in range(2):
            if hh == 1:
                P.barrier()
            P.op("pool", DMA(w_uq, d["a_w_uq"][:, hh * 384:(hh + 1) * 384].rearrange("(kc p) n -> p kc n", p=128)), writes=["w_uq"], dma=True)
            P.op("pool", DMA(w_ukv, d["a_w_ukv"][:, hh * 512:(hh + 1) * 512].rearrange("(kc p) n -> p kc n", p=128)), writes=["w_ukv"], dma=True)
            self.issue_casts(6)
            P.op("dve", MSET(Vaug[:, :, :, :], 1.0), writes=[("V", T) for T in range(18)])
            for T in range(18):
                tk = slice(T * 128, (T + 1) * 128)
                J = min(T // 4, 4)
                PK, PQ = _Rec(), _Rec()
                pk = 0 + self.rot("kvps", 2)
                psk = self.ps[pk]
                for kc in range(2):
                    PK.op("pe", MM(psk[:], kvlat[:, kc, tk], w_ukv[:, kc, :], kc == 0, kc == 1),
                         reads=[("kvlat", kc, J), "w_ukv"], writes=[("ps", pk)])
                psk3 = psk[:].rearrange("p (h e) -> p h e", h=4)
                k = "kw"
                sqf = self.tmpf[0][:, 256:512].rearrange("p (h e) -> p h e", h=4)
                ssk, rk = sm[:, 23, 0:4], sm[:, 23, 4:8]
                kn = self.tmpf[0][:, 0:256].rearrange("p (h e) -> p h e", h=4)
                PK.op("act", ACTV(sqf, psk3[:, :, 0:64], AF.Square), reads=[("ps", pk)], writes=[("tmpf", 0)])
                PK.op("dve", RSUM(ssk, sqf), reads=[("tmpf", 0)], writes=[k])
                PK.op("dve", TS1(ssk, ssk, sm[:, 20, T:T + 1], ALU.add), reads=[k, ("sskr", T)], writes=[k])
                PK.op("act", ACTV(rk, ssk, AF.Sqrt, bias=self.cst[:, 3:4]), reads=[k, "cst"], writes=[k])
                PK.op("dve", lambda e, rk=rk: e.reciprocal(out=rk, in_=rk), reads=[k], writes=[k])
                PK.op("dve", TT(kn, psk3[:, :, 0:64], rk.unsqueeze(2).broadcast_to([128, 4, 64]), ALU.mult),
                     reads=[("ps", pk), k], writes=[("tmpf", 0)])
                PK.op("dve", TT(self.kfin[:, :, 0:64], kn, self.kgB[:, 0:64].unsqueeze(1).broadcast_to([128, 4, 64]), ALU.mult),
                     reads=[("tmpf", 0), "kgB"], writes=["kfin"])
                PK.op("dve", TT(self.kfin[:, :, 64:96], krt[:, T, :].unsqueeze(1).broadcast_to([128, 4, 32]),
                               rk.unsqueeze(2).broadcast_to([128, 4, 32]), ALU.mult), reads=[("krt", T), k], writes=["kfin"])
                for par in range(2):
                    PK.op("act", ACTV(Vaug[:, T, par:4:2, par * 64:par * 64 + 64], psk3[:, par:4:2, 64:128], AF.Copy),
                         reads=[("ps", pk)], writes=[("V", T)])
                ptk = 4
                ptb = self.ps[ptk][:].bitcast(BF16)
                for hl in range(4):
                    PK.op("pe", TR(ptb[0:96, hl * 128:(hl + 1) * 128], self.kfin[:, hl, :], self.ident_b[:]),
                         reads=["kfin", "ident_b"], writes=[("ps", ptk)])
                PK.op("act", ACTV(KT[:, :, tk], ptb[0:96, 0:512].rearrange("p (h e) -> p h e", h=4), AF.Copy),
                     reads=[("ps", ptk)], writes=[("KT", T)])
                if T < 16:
                    pq = 2 + self.rot("qps", 2)
                    psq = self.ps[pq]
                    for kc in range(3):
                        PQ.op("pe", MM(psq[:, 0:384], qlat[:, kc, tk], w_uq[:, kc, :], kc == 0, kc == 2),
                             reads=[("qlat", kc, J), "w_uq"], writes=[("ps", pq)])
                    psq3 = psq[:, 0:384].rearrange("p (h e) -> p h e", h=4)
                    k = "qw"
                    sqq = self.rstdB[:, 0:384].rearrange("p (h e) -> p h e", h=4)
                    ssq, rq = sm[:, 23, 8:12], sm[:, 23, 12:16]
                    qn = self.tmpf[1][:, 0:384].rearrange("p (h e) -> p h e", h=4)
                    ta = sm[:, 16, :].bitcast(F32) if False else None
                    PQ.op("act", ACTV(sqq, psq3, AF.Square), reads=[("ps", pq)], writes=["rstdB"])
                    PQ.op("dve", RSUM(ssq, sqq), reads=["rstdB"], writes=[k])
                    PQ.op("act", ACTV(rq, ssq, AF.Sqrt, bias=self.cst[:, 3:4]), reads=[k, "cst"], writes=[k])
                    PQ.op("dve", lambda e, rq=rq: e.reciprocal(out=rq, in_=rq), reads=[k], writes=[k])
                    PQ.op("dve", TT(qn, psq3, rq.unsqueeze(2).broadcast_to([128, 4, 96]), ALU.mult),
                         reads=[("ps", pq), k], writes=[("tmpf", 1)])
                    PQ.op("dve", TT(qn, qn, self.qgB[:].unsqueeze(1).broadcast_to([128, 4, 96]), ALU.mult),
                         reads=[("tmpf", 1), "qgB"], writes=[("tmpf", 1)])
                    PQ.op("act", ACTV(self.qfin[:, :, 0:64], qn[:, :, 0:64], AF.Copy), reads=[("tmpf", 1)], writes=["qfin"])
                    cs = self.rope[:, T, 0:16].unsqueeze(1).broadcast_to([128, 4, 16])
                    sn = self.rope[:, T, 16:32].unsqueeze(1).broadcast_to([128, 4, 16])
                    u1 = sm[:, 0, 0:64].rearrange("p (h e) -> p h e", h=4) if False else self.tmpf[1][:, 384:448].rearrange("p (h e) -> p h e", h=4)
                    u2 = self.tmpf[1][:, 448:512].rearrange("p (h e) -> p h e", h=4)
                    kq = ("tmpf", 1)
                    PQ.op("dve", TT(u1, qn[:, :, 64:80], cs, ALU.mult), reads=[("tmpf", 1), "rope"], writes=[kq])
                    PQ.op("dve", TT(u2, qn[:, :, 80:96], sn, ALU.mult), reads=[("tmpf", 1), "rope"], writes=[kq])
                    PQ.op("dve", TT(self.qfin[:, :, 64:80], u1, u2, ALU.subtract), reads=[kq], writes=["qfin"])
                    PQ.op("dve", TT(u1, qn[:, :, 64:80], sn, ALU.mult), reads=[("tmpf", 1), "rope"], writes=[kq])
                    PQ.op("dve", TT(u2, qn[:, :, 80:96], cs, ALU.mult), reads=[("tmpf", 1), "rope"], writes=[kq])
                    PQ.op("dve", TT(self.qfin[:, :, 80:96], u1, u2, ALU.add), reads=[kq], writes=["qfin"])
                    ptq = 5
                    ptb = self.ps[ptq][:].bitcast(BF16)
                    for hl in range(4):
                        PQ.op("pe", TR(ptb[0:96, hl * 128:(hl + 1) * 128], self.qfin[:, hl, :], self.ident_b[:]),
                             reads=["qfin", "ident_b"], writes=[("ps", ptq)])
                    PQ.op("act", ACTV(QT[:, :, tk], ptb[0:96, 0:512].rearrange("p (h e) -> p h e", h=4), AF.Copy),
                         reads=[("ps", ptq)], writes=[("QT", T // 4)])

                for i_ in range(max(len(PK.l), len(PQ.l))):
                    if i_ < len(PK.l):
                        P.op(*PK.l[i_][0], **PK.l[i_][1])
                    if i_ < len(PQ.l):
                        P.op(*PQ.l[i_][0], **PQ.l[i_][1])
            for hl in range(4):
                h = hh * 4 + hl
                par = hl % 2
                orow = slice(par * 64, par * 64 + 64)
                drow = slice((1 - par) * 64, (1 - par) * 64 + 64)
                for jq in range(4):
                    tq = slice(jq * 512, (jq + 1) * 512)
                    po = 6 + self.rot("po", 2)
                    pso = self.ps[po]
                    pend = None
                    for kp in range(10):
                        if kp < 9:
                            pr = self.rot("sps2", 2)
                            b0 = 2 * pr
                            for q in range(2):
                                kt = 2 * kp + q
                                P.op("pe", MM(self.ps[b0 + q][:], KT[:, hl, kt * 128:(kt + 1) * 128], QT[:, hl, tq], True, True),
                                     reads=[("KT", kt), ("QT", jq)], writes=[("ps", b0 + q)])
                            rb = self.rot("tmpb2", 2)
                            P.op("act", ACTV(self.tmpb2[rb][:], self.psall[:, b0 * 512:(b0 + 2) * 512], AF.Exp,
                                             bias=self.negC[:], scale=math.sqrt(96.0)),
                                 reads=[("ps", b0), ("ps", b0 + 1), "negC"], writes=[("tmpb", 2 * rb), ("tmpb", 2 * rb + 1)])
                        if pend is not None:
                            pkp, prb = pend
                            for q in range(2):
                                pkt = 2 * pkp + q
                                P.op("pe", MM(pso[:], Vaug[:, pkt, hl, :], self.tmpb2[prb][:, q * 512:(q + 1) * 512], pkt == 0, pkt == 17),
                                     reads=[("V", pkt), ("tmpb", 2 * prb + q)], writes=[("ps", po)])
                        pend = (kp, rb) if kp < 9 else None
                    rf = self.rot("tmpf", 2)
                    rden = self.tmpf[rf]
                    P.op("dve", lambda e, rden=rden, pso=pso, orow=orow, drow=drow: e.reciprocal(out=rden[orow, :], in_=pso[drow, :]),
                         reads=[("ps", po)], writes=[("tmpf", rf)])
                    P.op("dve", TT(attnT[orow, h // 2, tq], pso[orow, :], rden[orow, :], ALU.mult),
                         reads=[("ps", po), ("tmpf", rf)], writes=[("attnT", h // 2, jq)])
        for j in range(4):
            tok = slice(j * 512, (j + 1) * 512)
            for m in range(8):
                py = 4 + self.rot("py0", 2)
                for kc in range(4):
                    P.op("pe", MM(self.ps[py][:], w_oa[:, kc, m * 128:(m + 1) * 128], attnT[:, kc, tok], kc == 0, kc == 3),
                         reads=["w_oa", ("attnT", kc, j)], writes=[("ps", py)])
                P.op("dve", STT(self.xT[:, m, tok], self.ps[py][:], g1(m), self.xT[:, m, tok], ALU.mult, ALU.add),
                     reads=[("ps", py), ("xT", m, j), "modT"], writes=[("xT", m, j)])


def _rope_tables():
    t = np.arange(S)
    row = (t // 64).astype(np.float32)
    col = (t % 64).astype(np.float32)
    inv = (np.float32(10000.0) ** (-np.arange(8, dtype=np.float32) / np.float32(8))).astype(np.float32)
    ang = np.concatenate([row[:, None] * inv, col[:, None] * inv], axis=-1).astype(np.float32)
    cs = np.concatenate([np.cos(ang), np.sin(ang)], axis=-1).astype(np.float32)
    return np.ascontiguousarray(cs.reshape(16, 128, 32).transpose(1, 0, 2))


def _chunks(v):
    v = np.asarray(v, np.float32).reshape(-1, 128)
    return v.T


def make_in_maps(inp, ncores=NCORES, nb=NB, sparse=False):
    f = lambda a: np.ascontiguousarray(np.asarray(a, np.float32))
    shared = dict(
        rope=_rope_tables(), ident=np.eye(128, dtype=np.float32),
        w_ada=f(inp["w_ada"]), a_w_in=f(inp["a_w_in"][0]), a_w_uq=f(inp["a_w_uq"][0]), a_w_ukv=f(inp["a_w_ukv"][0]),
        a_q_g=f(inp["a_q_g"]), a_k_g=f(inp["a_k_g"]), b_w_dw=f(inp["b_w_dw"][0]), ab_w_out=f(inp["ab_w_out"][0]),
        c_w_in=f(inp["c_w_in"][0]), c_ln_g=f(inp["c_ln_g"]), c_ln_b=f(inp["c_ln_b"]),
        w_sT=f(np.transpose(inp["c_w_s"][0], (2, 0, 1))), c_w_out=f(inp["c_w_out"][0]),
        wr=f(np.concatenate([inp["moe_w_group"], np.transpose(inp["moe_w_router"], (0, 2, 1, 3)).reshape(2, D, 32)], axis=-1)),
    )
    if sparse:
        g = np.asarray(inp["moe_w_gate"], np.float32).reshape(2, 32, 8, 128, 256)
        u = np.asarray(inp["moe_w_up"], np.float32).reshape(2, 32, 8, 128, 256)
        gu = np.concatenate([g, u], axis=-1)
        gu = np.ascontiguousarray(np.transpose(gu, (0, 1, 3, 2, 4))).reshape(2, 4096, 4096)
        shared["wgu_t0"], shared["wgu_t1"] = gu[0], gu[1]
        dn = np.asarray(inp["moe_w_down"], np.float32).reshape(2, 32, 2, 128, D)
        dn = np.ascontiguousarray(np.transpose(dn, (0, 1, 3, 2, 4))).reshape(2, 4096, 2048)
        shared["wd_t0"], shared["wd_t1"] = dn[0], dn[1]
        cst = np.zeros((128, 193), np.float32)
        cst[:, 0:64] = np.arange(64, dtype=np.float32)[None, :]
        cst[:, 64] = np.arange(128, dtype=np.float32)
        cst[:, 65:193] = np.triu(np.ones((128, 128), np.float32), 1)
        shared["consts"] = cst
    else:
        shared.update(moe_w_gate=f(inp["moe_w_gate"]), moe_w_up=f(inp["moe_w_up"]), moe_w_down=f(inp["moe_w_down"]))
    rows = np.zeros((1, NR), np.float32)
    rows[0, R_BDW:R_BDW + 512] = inp["b_b_dw"][0]
    rows[0, R_CBV:R_CBV + 2048] = inp["c_b_in"][0][2048:]
    rows[0, R_BS:R_BS + 1024] = np.asarray(inp["c_b_s"][0]).reshape(-1)
    for l in range(2):
        rows[0, R_MOEB + l * 36:R_MOEB + l * 36 + 4] = inp["moe_b_group"][l]
        rows[0, R_MOEB + l * 36 + 4:R_MOEB + (l + 1) * 36] = np.asarray(inp["moe_b_router"][l]).reshape(-1)
    shared["rows"] = rows
    maps = []
    for ci in range(ncores):
        bs = [ci * nb + k for k in range(nb)]
        vecs = np.zeros((128, NV), np.float32)
        cc = np.zeros((128, 8, 3), np.float32)
        for k, b in enumerate(bs):
            cc[:, :, k] = _chunks(inp["c"][b])
        cc[:, :, 2] = _chunks(inp["c_ctx"])
        if nb == 1:
            cc[:, :, 1] = cc[:, :, 0]
        vecs[:, V_C:V_C + 24] = cc.reshape(128, 24)
        for l in range(2):
            vecs[:, V_BADA + l * 48:V_BADA + (l + 1) * 48] = _chunks(inp["b_ada"][l])
            vecs[:, V_N1G + l * 8:V_N1G + (l + 1) * 8] = _chunks(inp["norm1_g"][l])
            vecs[:, V_N2G + l * 8:V_N2G + (l + 1) * 8] = _chunks(inp["norm2_g"][l])
        vecs[:, V_QNG:V_QNG + 3] = _chunks(inp["a_q_norm_g"][0])
        vecs[:, V_KVNG:V_KVNG + 2] = _chunks(inp["a_kv_norm_g"][0])
        vecs[:, V_BLNG:V_BLNG + 4] = _chunks(inp["b_ln_g"][0])
        vecs[:, V_BLNB:V_BLNB + 4] = _chunks(inp["b_ln_b"][0])
        vecs[:, V_CBU:V_CBU + 16] = _chunks(inp["c_b_in"][0][:2048])
        vecs[:, V_DW:V_DW + 124] = np.transpose(np.asarray(inp["b_w_dw"][0], np.float32).reshape(31, 4, 128), (2, 1, 0)).reshape(128, 124)
        m = dict(shared)
        m["vecs"] = vecs
        m["xT"] = np.ascontiguousarray(np.transpose(np.asarray(inp["x"], np.float32)[bs], (0, 2, 1)))
        m["ctxT"] = np.ascontiguousarray(np.transpose(np.asarray(inp["ctx"], np.float32)[bs], (0, 2, 1)))
        maps.append(m)
    return maps


_PROG_CACHE = {}


def get_prog(phases, nb):
    key = (tuple(phases), nb)
    if key not in _PROG_CACHE:
        _PROG_CACHE[key] = KB(list(phases), nb).nc
    return _PROG_CACHE[key]


def kernel(**inputs):
    phases = ["mix0", "smoe0", "mix1", "smoe1"]
    nc = get_prog(phases, NB)
    maps = make_in_maps(inputs, sparse=True)
    res = run_bass_kernel_spmd(nc, maps, core_ids=list(range(NCORES)))
    out = np.empty((16, S, D), np.float32)
    for ci in range(NCORES):
        o = res.results[ci]["outT"]
        for k in range(NB):
            out[ci * NB + k] = o[k].T
    return out
```

```python
import contextlib
import math
import numpy as np
import concourse.bass as bass
import concourse.mybir as mybir
from concourse.bass_utils import run_bass_kernel_spmd

F32 = mybir.dt.float32
BF16 = mybir.dt.bfloat16
AF = mybir.ActivationFunctionType
ALU = mybir.AluOpType
AX = mybir.AxisListType

D = 1024
S = 2048
CTX = 256
NCORES = 8
NB = 2

V_C, V_BADA, V_N1G, V_N2G, V_QNG, V_KVNG, V_BLNG, V_BLNB, V_CBU = 0, 24, 120, 136, 152, 155, 157, 161, 165
V_DW = 181
NV = 305
R_BDW, R_CBV, R_BS, R_MOEB = 0, 512, 2560, 3584
NR = 3656
ARENA = 53000
STRICT_SAME_ENGINE = False


class Prog:
    ENGS = ("pe", "act", "dve", "pool", "sp")
    N_DMA_SEMS = 16

    def __init__(self, nc):
        self.nc = nc
        self.ops = []
        self.last_w = {}
        self.readers = {}

    def op(self, eng, fn, reads=(), writes=(), dma=False, semq=None):
        deps = {}
        for k in reads:
            w = self.last_w.get(k)
            if w is not None:
                deps[w] = True
        for k in writes:
            w = self.last_w.get(k)
            if w is not None:
                deps.setdefault(w, False)
            for r in self.readers.get(k, ()):
                deps.setdefault(r, False)
        i = len(self.ops)
        self.ops.append(dict(eng=eng, fn=fn, deps=deps, dma=dma, barrier=False, semq=semq))
        for k in reads:
            self.readers.setdefault(k, []).append(i)
        for k in writes:
            self.last_w[k] = i
            self.readers[k] = []
        return i

    def barrier(self):
        self.ops.append(dict(eng=None, fn=None, deps={}, dma=False, barrier=True))

    def emit(self):
        nc = self.nc
        ops = self.ops
        signal = [False] * len(ops)
        for i, o in enumerate(ops):
            if o["barrier"]:
                continue
            enf = []
            for d, raw in o["deps"].items():
                od = ops[d]
                if od["dma"]:
                    enf.append(d)
                    continue
                if od["eng"] == o["eng"] and not o["dma"]:
                    if o["eng"] == "pe" or (not raw and not STRICT_SAME_ENGINE):
                        continue
                enf.append(d)
                signal[d] = True
            o["enf"] = enf
        last = {}
        for i, o in enumerate(ops):
            if o["barrier"]:
                o["last"] = dict(last)
                for e, j in last.items():
                    signal[j] = True
            elif not o["dma"]:
                last[o["eng"]] = i
        cnt = {e: 0 for e in self.ENGS}
        for i, o in enumerate(ops):
            if o["barrier"] or o["dma"]:
                continue
            if signal[i]:
                cnt[o["eng"]] += 1
                o["sig"] = cnt[o["eng"]]
        with contextlib.ExitStack() as st:
            sems = {e: st.enter_context(nc.semaphore("s_" + e)) for e in ("pe", "act", "dve", "pool")}
            nsem = {"sp": self.N_DMA_SEMS, "act": self.N_DMA_SEMS, "pool": self.N_DMA_SEMS, "bg": 28}
            dsems = {q: [st.enter_context(nc.semaphore(f"d_{q}{j}")) for j in range(n)] for q, n in nsem.items()}
            dcount = {q: 0 for q in dsems}
            dtarget = {(q, j): 0 for q in dsems for j in range(nsem[q])}
            for i, o in enumerate(ops):
                if o["barrier"]:
                    o["dsnap"] = {k_: v_ for k_, v_ in dtarget.items() if k_[0] != "bg"}
                elif o["dma"]:
                    q = o.get("semq") or o["eng"]
                    j = dcount[q] % nsem[q]
                    dcount[q] += 1
                    o["dq"] = (q, j)
                    o["dprev"] = dtarget[(q, j)]
                    dtarget[(q, j)] += 16
                    o["dtgt"] = dtarget[(q, j)]
            block = st.enter_context(nc.Block())

            def stream(engname):
                def body(e):
                    seen = {}
                    mydma = {}

                    def wait(key, sem, val):
                        if val > 0 and seen.get(key, 0) < val:
                            e.wait_ge(sem, val)
                            seen[key] = val

                    for i, o in enumerate(ops):
                        if o["barrier"]:
                            for en, j in o["last"].items():
                                if en != engname:
                                    wait(en, sems[en], ops[j]["sig"])
                            for (q, j), t in o["dsnap"].items():
                                wait((q, j), dsems[q][j], t)
                            continue
                        if o["eng"] != engname:
                            continue
                        for d in o["enf"]:
                            od = ops[d]
                            if od["dma"]:
                                q, j = od["dq"]
                                wait((q, j), dsems[q][j], od["dtgt"])
                            else:
                                wait(od["eng"], sems[od["eng"]], od["sig"])
                        if o["dma"]:
                            q, j = o["dq"]
                            wait((q, j), dsems[q][j], o["dprev"])
                            ins = o["fn"](e)
                            ins.then_inc(dsems[q][j], 16)
                            mydma[(q, j)] = o["dtgt"]
                        else:
                            ins = o["fn"](e)
                            if signal[i]:
                                ins.then_inc(sems[engname], 1)
                    for (q, j), t in mydma.items():
                        wait((q, j), dsems[q][j], t)
                return body

            used = {o["eng"] for o in ops if not o["barrier"]}
            if "pe" in used:
                block.tensor(stream("pe"))
            if "act" in used:
                block.scalar(stream("act"))
            if "dve" in used:
                block.vector(stream("dve"))
            if "pool" in used:
                block.gpsimd(stream("pool"))
            if "sp" in used:
                block.sync(stream("sp"))


def MM(out, lhsT, rhs, start, stop):
    return lambda e: e.matmul(out, lhsT=lhsT, rhs=rhs, start=start, stop=stop)


def TR(out, in_, ident):
    return lambda e: e.transpose(out=out, in_=in_, identity=ident)


def ACTV(out, in_, func, bias=None, scale=None):
    kw = {}
    if bias is not None:
        kw["bias"] = bias
    if scale is not None:
        kw["scale"] = scale
    return lambda e: e.activation(out=out, in_=in_, func=func, **kw)


def TT(out, in0, in1, op):
    return lambda e: e.tensor_tensor(out=out, in0=in0, in1=in1, op=op)


def TS(out, in0, s1, s2, op0, op1):
    return lambda e: e.tensor_scalar(out=out, in0=in0, scalar1=s1, scalar2=s2, op0=op0, op1=op1)


def TS1(out, in_, s, op):
    return lambda e: e.tensor_single_scalar(out=out, in_=in_, scalar=s, op=op)


def STT(out, in0, scalar, in1, op0, op1):
    return lambda e: e.scalar_tensor_tensor(out=out, in0=in0, scalar=scalar, in1=in1, op0=op0, op1=op1)


def CP(out, in_):
    return lambda e: e.tensor_copy(out=out, in_=in_)


def DMA(out, in_):
    return lambda e: e.dma_start(out=out, in_=in_)


def RMAX(out, in_):
    return lambda e: e.reduce_max(out=out, in_=in_, axis=AX.X)


def RSUM(out, in_):
    return lambda e: e.reduce_sum(out=out, in_=in_, axis=AX.X)


def MSET(ap, v):
    return lambda e: e.memset(ap, v)


class _Rec:
    def __init__(self):
        self.l = []

    def op(self, *a, **k):
        self.l.append((a, k))


class KB:
    def __init__(self, phases, nb=NB, debug=None):
        self.phases = phases
        self.nb = nb
        nc = self.nc = bass.Bass("TRN2", target_bir_lowering=False)
        self.st = contextlib.ExitStack()
        din = lambda name, shape: nc.dram_tensor(name, list(shape), F32, kind="ExternalInput").ap()
        self.sparse = any(p.startswith("smoe") for p in phases)
        self.d = dict(
            xT=din("xT", [nb, D, S]), ctxT=din("ctxT", [nb, D, CTX]), vecs=din("vecs", [128, NV]),
            rows=din("rows", [1, NR]), rope=din("rope", [128, 16, 32]), ident=din("ident", [128, 128]),
            w_ada=din("w_ada", [2, D, 6 * D]), a_w_in=din("a_w_in", [D, 1696]), a_w_uq=din("a_w_uq", [384, 768]),
            a_w_ukv=din("a_w_ukv", [256, 1024]), a_q_g=din("a_q_g", [1, 96]), a_k_g=din("a_k_g", [1, 96]),
            b_w_dw=din("b_w_dw", [31, 512]), ab_w_out=din("ab_w_out", [D, D]),
            c_w_in=din("c_w_in", [D, 4096]), c_ln_g=din("c_ln_g", [1, 2048]), c_ln_b=din("c_ln_b", [1, 2048]),
            w_sT=din("w_sT", [128, 8, 128]), c_w_out=din("c_w_out", [2048, D]),
            wr=din("wr", [2, D, 36]),
        )
        if self.sparse:
            self.d.update(consts=din("consts", [128, 193]),
                          wgu_t=[din(f"wgu_t{i}", [4096, 4096]) for i in range(2)],
                          wd_t=[din(f"wd_t{i}", [4096, 2048]) for i in range(2)])
            self.hs = nc.dram_tensor("hs_scr", [S, D], BF16, kind="Internal").ap()
            self.SL = nc.dram_tensor("sl_scr", [8192, 4], F32, kind="Internal").ap()
            self.Y = nc.dram_tensor("y_scr", [2 * S, D], F32, kind="Internal").ap()
            self.wgu_b = [nc.dram_tensor(f"wgu_b{i}", [4096, 4096], BF16, kind="Internal").ap() for i in range(2)]
            self.wd_b = [nc.dram_tensor(f"wd_b{i}", [4096, 2048], BF16, kind="Internal").ap() for i in range(2)]
            self.cwin_b = nc.dram_tensor("cwin_b", [D, 4096], BF16, kind="Internal").ap()
            self.cwout_b = nc.dram_tensor("cwout_b", [2048, D], BF16, kind="Internal").ap()
            self.cast_jobs = [
                (0, self.cwin_b[0:512, :], self.d["c_w_in"][0:512, :], ("cwcast", 0)),
                (0, self.cwin_b[512:1024, :], self.d["c_w_in"][512:1024, :], ("cwcast", 1)),
                (0, self.cwout_b[0:1024, :], self.d["c_w_out"][0:1024, :], ("cwcast", 2)),
                (0, self.cwout_b[1024:2048, :], self.d["c_w_out"][1024:2048, :], ("cwcast", 3)),
            ]
            for i in range(2):
                for q in range(8):
                    self.cast_jobs.append((i, self.wgu_b[i][q * 512:(q + 1) * 512, :], self.d["wgu_t"][i][q * 512:(q + 1) * 512, :], ("wcast", i, q)))
                for q in range(4):
                    self.cast_jobs.append((i, self.wd_b[i][q * 1024:(q + 1) * 1024, :], self.d["wd_t"][i][q * 1024:(q + 1) * 1024, :], ("wcast", i, 8 + q)))
        else:
            self.d.update(moe_w_gate=din("moe_w_gate", [2, 32, D, 256]), moe_w_up=din("moe_w_up", [2, 32, D, 256]),
                          moe_w_down=din("moe_w_down", [2, 32, 256, D]))
        self.outT = nc.dram_tensor("outT", [nb, D, S], F32, kind="ExternalOutput").ap()
        self.P = Prog(nc)
        self._n = 0
        self.build()

    def sb(self, shape, dt, name=None):
        self._n += 1
        return self.st.enter_context(self.nc.sbuf_tensor("sb_" + (name or f"t{self._n}"), list(shape), dt))

    def av(self, off, shape, dt=BF16, parts=128):
        n = int(np.prod(shape))
        if dt == F32:
            ap = self.arena[0:parts, off:off + 2 * n].bitcast(F32)
        else:
            ap = self.arena[0:parts, off:off + n]
        assert off + (2 * n if dt == F32 else n) <= ARENA, (off, shape)
        if len(shape) == 2:
            return ap.rearrange("p (a b) -> p a b", a=shape[0])
        if len(shape) == 3:
            return ap.rearrange("p (a b c) -> p a b c", a=shape[0], b=shape[1])
        return ap

    def build(self):
        nc, P, d = self.nc, self.P, self.d
        with self.st:
            self.xT = self.sb([128, 8, S], F32, "xT")
            self.arena = self.sb([128, ARENA], BF16, "arena")
            self.psall = self.st.enter_context(nc.psum_tensor("psall", [128, 4096], F32))
            self.ps = [self.psall[:, i * 512:(i + 1) * 512] for i in range(8)]
            self.vecs = self.sb([128, NV], F32, "vecs")
            self.modT = self.sb([128, 2, 48, 3], F32, "modT")
            self.scl1 = self.sb([128, 2, 8, 3], F32, "scl1")
            self.scl2 = self.sb([128, 2, 8, 3], F32, "scl2")
            self.ident_f = self.sb([128, 128], F32, "ident_f")
            self.ident_b = self.sb([128, 128], BF16, "ident_b")
            self.ones_b = self.sb([128, 128], BF16, "ones_b")
            self.ones_f = self.sb([1, 128], F32, "ones_f")
            self.kfin = self.sb([128, 4, 96], BF16, "kfin")
            self.qfin = self.sb([128, 4, 96], BF16, "qfin")
            self.rows_f = self.sb([1, 72], F32, "rows_f")
            self.rope = self.sb([128, 16, 32], F32, "rope")
            self.sel = self.sb([32, 32, 128], BF16, "sel")
            self.qgB = self.sb([128, 96], F32, "qgB")
            self.kgB = self.sb([128, 96], F32, "kgB")
            self.qkng = self.sb([128, 5], F32, "qkng")
            self.negC = self.sb([128, 1], F32, "negC")
            self.wr = self.sb([128, 2, 8, 36], F32, "wr")
            self.scT = self.sb([128, 24], BF16, "scT")
            self.sq = [self.sb([128, 512], BF16, f"sq{i}") for i in range(2)]
            self.rstdB = self.sb([128, 512], F32, "rstdB")
            self.tmpf = [self.sb([128, 512], F32, f"tmpf{i}") for i in range(2)]
            self.tmpb2 = [self.sb([128, 1024], BF16, f"tmpb2_{i}") for i in range(2)]
            self.tmpb = [self.tmpb2[i // 2][:, (i % 2) * 512:(i % 2 + 1) * 512] for i in range(4)]
            self.sm = self.sb([128, 24, 36], F32, "sm")
            self.cst = self.sb([128, 8], F32, "cst")
            if self.sparse:
                self.iota = self.sb([128, 65], F32, "iota")
                self.U_b = self.sb([128, 128], BF16, "U_b")
            self._cnt = {}
            self.setup()
            for bi in range(self.nb):
                self.load_x(bi)
                for ph in self.phases:
                    P.barrier()
                    if ph == "mix0":
                        self.phase_mix0(bi)
                    elif ph == "moe0":
                        self.phase_moe(0, bi)
                    elif ph == "mix1":
                        self.phase_mix1(bi)
                    elif ph == "moe1":
                        self.phase_moe(1, bi)
                    elif ph == "smoe0":
                        self.phase_smoe(0, bi)
                    elif ph == "smoe1":
                        self.phase_smoe(1, bi)
                P.barrier()
                self.store_x(bi)
            P.emit()

    def issue_casts(self, n=None, layer=None):
        if not self.sparse:
            return
        k = 0
        while self.cast_jobs and (n is None or k < n):
            if layer is not None and self.cast_jobs[0][0] > layer:
                break
            i, dst, src, key = self.cast_jobs.pop(0)
            self.P.op("pool", DMA(dst, src), writes=[key], dma=True, semq="bg")
            k += 1

    def rot(self, name, n):
        c = self._cnt.get(name, 0)
        self._cnt[name] = c + 1
        return c % n

    def setup(self):
        P, d = self.P, self.d
        P.op("sp", DMA(self.vecs[:], d["vecs"]), writes=["vecs"], dma=True)
        P.op("sp", DMA(self.ident_f[:], d["ident"]), writes=["ident_f"], dma=True)
        P.op("pool", DMA(self.ident_b[:], d["ident"]), writes=["ident_b"], dma=True)
        P.op("sp", DMA(self.rows_f[:], d["rows"][:, R_MOEB:R_MOEB + 72]), writes=["rows_f"], dma=True)
        P.op("sp", DMA(self.rope[:], d["rope"]), writes=["rope"], dma=True)
        P.op("sp", DMA(self.qgB[:], d["a_q_g"][0].partition_broadcast(128)), writes=["qgB"], dma=True)
        P.op("sp", DMA(self.kgB[:], d["a_k_g"][0].partition_broadcast(128)), writes=["kgB"], dma=True)
        for l in range(2):
            P.op("sp", DMA(self.wr[:, l], d["wr"][l].rearrange("(kc p) n -> p kc n", p=128)), writes=["wr"], dma=True)
        if self.sparse:
            P.op("sp", DMA(self.iota[:], d["consts"][:, 0:65]), writes=["iota"], dma=True)
            P.op("pool", DMA(self.U_b[:], d["consts"][:, 65:193]), writes=["U_b"], dma=True)
        P.op("dve", MSET(self.ones_b[:], 1.0), writes=["ones_b"])
        for ci, v in enumerate((1024e-6, 384e-6, 256e-6, 96e-6, 1e-5, 0.0)):
            P.op("dve", MSET(self.cst[:, ci:ci + 1], v), writes=["cst"])
        P.op("dve", MSET(self.ones_f[:], 1.0), writes=["ones_f"])
        P.op("dve", CP(self.sel[:], self.ident_b[0:32, 0:32].unsqueeze(2).broadcast_to([32, 32, 128])),
             reads=["ident_b"], writes=["sel"])
        P.op("act", ACTV(self.scT[:], self.vecs[:, V_C:V_C + 24], AF.Silu), reads=["vecs"], writes=["scT"])
        psm = self.ps[0]
        for l in range(2):
            for s in range(12):
                slot = self.rot("wada", 4)
                wb = self.av(slot * 4096, [8, 512])
                P.op("pool", DMA(wb, self.d["w_ada"][l][:, s * 512:(s + 1) * 512].rearrange("(kc p) n -> p kc n", p=128)),
                     writes=[("wada", slot)], dma=True)
                for q in range(4):
                    ch = s * 4 + q
                    for kc in range(8):
                        P.op("pe", MM(psm[:, ch * 3:ch * 3 + 3], wb[:, kc, q * 128:(q + 1) * 128],
                                      self.scT[:, kc * 3:kc * 3 + 3], kc == 0, kc == 7),
                             reads=[("wada", slot), "scT"], writes=["ps0"])
            P.op("dve", TT(self.modT[:, l], psm[:, 0:144].rearrange("p (a b) -> p a b", a=48),
                           self.vecs[:, V_BADA + l * 48:V_BADA + (l + 1) * 48].unsqueeze(2).broadcast_to([128, 48, 3]),
                           ALU.add), reads=["ps0", "vecs"], writes=["modT"])
            for (scl, c0, vg) in ((self.scl1, 8, V_N1G), (self.scl2, 32, V_N2G)):
                P.op("dve", TS(scl[:, l], self.modT[:, l, c0:c0 + 8, :], 1.0, 32.0, ALU.add, ALU.mult),
                     reads=["modT"], writes=["scl"])
                P.op("dve", TT(scl[:, l], scl[:, l],
                               self.vecs[:, vg + l * 8:vg + (l + 1) * 8].unsqueeze(2).broadcast_to([128, 8, 3]), ALU.mult),
                     reads=["scl", "vecs"], writes=["scl"])
        P.op("dve", TS1(self.qkng[:, 0:3], self.vecs[:, V_QNG:V_QNG + 3], math.sqrt(384.0), ALU.mult),
             reads=["vecs"], writes=["qkng"])
        P.op("dve", TS1(self.qkng[:, 3:5], self.vecs[:, V_KVNG:V_KVNG + 2], 16.0, ALU.mult),
             reads=["vecs", "qkng"], writes=["qkng"])
        t = self.sm
        P.op("dve", TT(self.tmpf[0][:, 0:96], self.qgB[:], self.qgB[:], ALU.mult),
             reads=["qgB"], writes=["tmpf0"])
        P.op("dve", RMAX(t[:, 0, 0:1], self.tmpf[0][:, 0:96]), reads=["tmpf0"], writes=["sm0"])
        P.op("dve", TT(self.tmpf[1][:, 0:96], self.kgB[:], self.kgB[:], ALU.mult), reads=["kgB"], writes=["tmpf1"])
        P.op("dve", RMAX(t[:, 0, 1:2], self.tmpf[1][:, 0:96]), reads=["tmpf1", "sm0"], writes=["sm0"])
        P.op("dve", TT(t[:, 0, 2:3], t[:, 0, 0:1], t[:, 0, 1:2], ALU.mult), reads=["sm0"], writes=["sm0"])
        P.op("act", ACTV(t[:, 0, 3:4], t[:, 0, 2:3], AF.Sqrt), reads=["sm0"], writes=["sm0"])
        P.op("dve", TS1(self.negC[:], t[:, 0, 3:4], -math.sqrt(96.0), ALU.mult), reads=["sm0"], writes=["negC"])

    def load_x(self, bi):
        P = self.P
        for j in range(4):
            tok = slice(j * 512, (j + 1) * 512)
            for c in range(8):
                P.op("sp", DMA(self.xT[:, c, tok], self.d["xT"][bi, c * 128:(c + 1) * 128, tok]),
                     writes=[("xT", c, j)], dma=True, semq="bg")

    def store_x(self, bi):
        P = self.P
        for j in range(4):
            tok = slice(j * 512, (j + 1) * 512)
            for c in range(8):
                P.op("sp", DMA(self.outT[bi, c * 128:(c + 1) * 128, tok], self.xT[:, c, tok]),
                     reads=[("xT", c, j)], dma=True, semq="bg")

    def rmsnorm_T(self, srcs, ntok, neps, scales, shifts, dsts, psb, extra=None):
        P = self.P
        n = len(srcs)
        ps = self.ps[psb]
        for c, (src, skey) in enumerate(srcs):
            r = self.rot("sq", 2)
            P.op("act", ACTV(self.sq[r][:, 0:ntok], src, AF.Square), reads=[skey], writes=[("sq", r)])
            P.op("pe", MM(ps[:, 0:ntok], self.ones_b[:], self.sq[r][:, 0:ntok], c == 0, c == n - 1),
                 reads=[("sq", r), "ones_b"], writes=[("ps", psb)])
        P.op("act", ACTV(self.rstdB[:, 0:ntok], ps[:, 0:ntok], AF.Sqrt, bias=self.cst[:, neps:neps + 1]),
             reads=[("ps", psb), "cst"], writes=["rstdB"])
        P.op("dve", lambda e: e.reciprocal(out=self.rstdB[:, 0:ntok], in_=self.rstdB[:, 0:ntok]),
             reads=["rstdB"], writes=["rstdB"])
        for c, (src, skey) in enumerate(srcs):
            dst, dkey = dsts[c]
            if shifts is None:
                P.op("dve", STT(dst, src, scales[c], self.rstdB[:, 0:ntok], ALU.mult, ALU.mult),
                     reads=[skey, "rstdB"], writes=[dkey])
            else:
                r = self.rot("tmpf", 2)
                P.op("dve", STT(self.tmpf[r][:, 0:ntok], src, scales[c], self.rstdB[:, 0:ntok], ALU.mult, ALU.mult),
                     reads=[skey, "rstdB"], writes=[("tmpf", r)])
                P.op("act", ACTV(dst, self.tmpf[r][:, 0:ntok], AF.Identity, bias=shifts[c]),
                     reads=[("tmpf", r)], writes=[dkey])
                if extra is not None:
                    extra(c, dst, dkey)

    def phase_moe(self, l, bi):
        P, d = self.P, self.d
        G = 2
        h2T = self.av(0, [8, S])
        wgu = [self.av(16384 + s * 4096, [2, 8, 256]) for s in range(3)]
        wdn = [self.av(28672 + s * 2048, [2, 1024]) for s in range(4)]
        actT = [self.av(36864 + g * 4096, [2, S]) for g in range(G)]
        h2f = self.av(36864, [8, 512], F32)
        dwT = self.av(45056, [S], parts=32)
        sm = self.sm
        g2 = lambda m: self.modT[:, l, 40 + m, bi:bi + 1]
        for j in range(4):
            tok = slice(j * 512, (j + 1) * 512)
            srcs = [(self.xT[:, c, tok], ("xT", c, j)) for c in range(8)]
            dsts = [(h2f[:, c, :], ("h2f", c)) for c in range(8)]
            scales = [self.scl2[:, l, c, bi:bi + 1] for c in range(8)]
            shifts = [self.modT[:, l, 24 + c, bi:bi + 1] for c in range(8)]

            def extra(c, dst, dkey, j=j, tok=tok):
                P.op("pool", CP(h2T[:, c, tok], dst), reads=[dkey], writes=[("h2T", c, j)])
            self.rmsnorm_T(srcs, 512, 0, scales, shifts, dsts, 7, extra)
            for t in range(4):
                tt = slice(t * 128, (t + 1) * 128)
                pb = 5 + self.rot("rt_ps", 2)
                psl = self.ps[pb]
                P.op("pe", MM(psl[:, 0:36], self.ones_f[0:1, :], self.rows_f[0:1, l * 36:(l + 1) * 36], True, False),
                     reads=["ones_f", "rows_f"], writes=[("ps", pb)])
                for kc in range(8):
                    P.op("pe", MM(psl[:, 0:36], h2f[:, kc, tt], self.wr[:, l, kc, :], False, kc == 7),
                         reads=[("h2f", kc), "wr"], writes=[("ps", pb)])
                k = "rt"
                lg = sm[:, 0, :]
                P.op("act", ACTV(lg, psl[:, 0:36], AF.Copy), reads=[("ps", pb)], writes=[k])
                gmax, ngmax, gs, gw = sm[:, 1, 0:1], sm[:, 1, 1:2], sm[:, 1, 2:3], sm[:, 1, 3:4]
                m1, m2, dn, w1, w2 = sm[:, 1, 4:5], sm[:, 1, 5:6], sm[:, 1, 6:7], sm[:, 1, 7:8], sm[:, 1, 8:9]
                ohg, pen, eg = sm[:, 2, 0:4], sm[:, 2, 4:8], sm[:, 2, 8:12]
                elm, oh1, elm2, oh2, dw = sm[:, 3, 0:32], sm[:, 4, 0:32], sm[:, 5, 0:32], sm[:, 6, 0:32], sm[:, 7, 0:32]
                o = lambda eng, fn: P.op(eng, fn, reads=[k], writes=[k])
                o("dve", RMAX(gmax, lg[:, 0:4]))
                o("dve", TS1(ohg, lg[:, 0:4], gmax, ALU.is_equal))
                o("dve", TS1(ngmax, gmax, -1.0, ALU.mult))
                o("act", ACTV(eg, lg[:, 0:4], AF.Exp, bias=ngmax))
                o("dve", RSUM(gs, eg))
                o("dve", lambda e: e.reciprocal(out=gw, in_=gs))
                o("dve", TS(pen, ohg, 1.0, 1.0e4, ALU.subtract, ALU.mult))
                o("dve", TT(elm.rearrange("p (a b) -> p a b", a=4), lg[:, 4:36].rearrange("p (a b) -> p a b", a=4),
                            pen.unsqueeze(2).broadcast_to([128, 4, 8]), ALU.add))
                o("dve", RMAX(m1, elm))
                o("dve", TS1(oh1, elm, m1, ALU.is_equal))
                o("dve", STT(elm2, oh1, -1.0e4, elm, ALU.mult, ALU.add))
                o("dve", RMAX(m2, elm2))
                o("dve", TS1(oh2, elm2, m2, ALU.is_equal))
                o("dve", TT(dn, m2, m1, ALU.subtract))
                o("act", ACTV(w2, dn, AF.Sigmoid))
                o("act", ACTV(w1, dn, AF.Sigmoid, scale=-1.0))
                o("dve", TT(w1, w1, gw, ALU.mult))
                o("dve", TT(w2, w2, gw, ALU.mult))
                o("dve", TS1(dw, oh1, w1, ALU.mult))
                o("dve", STT(dw, oh2, w2, dw, ALU.mult, ALU.add))
                pt = self.ps[pb]
                P.op("pe", TR(pt[0:32, 128:256], dw, self.ident_f[:]), reads=[k, "ident_f"], writes=[("ps", pb)])
                P.op("act", ACTV(dwT[0:32, j * 512 + t * 128:j * 512 + (t + 1) * 128], pt[0:32, 128:256], AF.Copy),
                     reads=[("ps", pb)], writes=[("dwT", j)])
        P.barrier()
        for e in range(32):
            s3 = e % 3
            s4 = e % 4
            wg_, wu_ = wgu[s3][:, 0], wgu[s3][:, 1]
            P.op("pool", DMA(wg_, d["moe_w_gate"][l, e].rearrange("(kc p) n -> p kc n", p=128)),
                 writes=[("wg", s3)], dma=True)
            P.op("pool", DMA(wu_, d["moe_w_up"][l, e].rearrange("(kc p) n -> p kc n", p=128)),
                 writes=[("wu", s3)], dma=True)
            P.op("pool", DMA(wdn[s4], d["moe_w_down"][l, e].rearrange("(kc p) n -> p kc n", p=128)),
                 writes=[("wd", s4)], dma=True)
            ge = e % G
            for j in range(4):
                tok = slice(j * 512, (j + 1) * 512)
                pw = 4 + self.rot("pw", 2)
                P.op("pe", MM(self.ps[pw][:], self.sel[0:32, e, :], dwT[0:32, tok], True, True),
                     reads=["sel", ("dwT", j)], writes=[("ps", pw)])
                for c in range(2):
                    r = self.rot("gu", 2)
                    pg, pu = 0 + r, 2 + r
                    for kc in range(8):
                        P.op("pe", MM(self.ps[pg][:], wg_[:, kc, c * 128:(c + 1) * 128], h2T[:, kc, tok], kc == 0, kc == 7),
                             reads=[("wg", s3), ("h2T", kc, j)], writes=[("ps", pg)])
                    for kc in range(8):
                        P.op("pe", MM(self.ps[pu][:], wu_[:, kc, c * 128:(c + 1) * 128], h2T[:, kc, tok], kc == 0, kc == 7),
                             reads=[("wu", s3), ("h2T", kc, j)], writes=[("ps", pu)])
                    rb = self.rot("tmpb", 2)
                    sg, tb = self.tmpb[rb], self.tmpb[2 + rb]
                    P.op("act", ACTV(sg[:], self.ps[pg][:], AF.Silu), reads=[("ps", pg)], writes=[("tmpb", rb)])
                    P.op("dve", TT(tb[:], sg[:], self.ps[pu][:], ALU.mult), reads=[("tmpb", rb), ("ps", pu)],
                         writes=[("tmpb", 2 + rb)])
                    P.op("dve", TT(actT[ge][:, c, tok], tb[:], self.ps[pw][:], ALU.mult),
                         reads=[("tmpb", 2 + rb), ("ps", pw)], writes=[("actT", ge, c, j)])
            if ge == G - 1:
                for j in range(4):
                    tok = slice(j * 512, (j + 1) * 512)
                    for m in range(8):
                        py = 6 + self.rot("py", 2)
                        n = 0
                        for gg in range(G):
                            ee = e - (G - 1) + gg
                            for kc in range(2):
                                P.op("pe", MM(self.ps[py][:], wdn[ee % 4][:, kc, m * 128:(m + 1) * 128], actT[gg][:, kc, tok],
                                              n == 0, n == 2 * G - 1),
                                     reads=[("wd", ee % 4), ("actT", gg, kc, j)], writes=[("ps", py)])
                                n += 1
                        P.op("dve", STT(self.xT[:, m, tok], self.ps[py][:], g2(m), self.xT[:, m, tok], ALU.mult, ALU.add),
                             reads=[("ps", py), ("xT", m, j), "modT"], writes=[("xT", m, j)])

    def phase_smoe(self, l, bi):
        P, d = self.P, self.d
        I32 = mybir.dt.int32
        sm = self.sm
        av = self.av
        g2 = lambda m: self.modT[:, l, 40 + m, bi:bi + 1]
        iota_r, iota_p = self.iota[:, 0:64], self.iota[:, 64:65]
        NT = 63
        h2f_ = [av(0, [8, 512], F32), av(25600, [8, 512], F32)]
        h2b_ = [av(8192, [8, 512]), av(33792, [8, 512])]
        hrow = [av(12288 + i * 1024, [1024]) for i in range(2)]
        oh_all = av(14336, [16, 2, 32], F32)
        pre_all = av(16384, [16, 32], F32)
        pos_all = av(17408, [16, 32], F32)
        wts_all = av(18432, [16, 2], F32)
        vals_all = av(18496, [16, 2, 4], F32)
        slot_f = av(18752, [16, 2], F32)
        slot_i = av(18816, [16, 2], I32 if False else F32).bitcast(I32)
        cmp = av(18880, [64, 32], F32)
        te = av(22976, [64], F32)
        ohcum = av(23232, [32])
        base_r = av(23392, [32], F32)
        nt_r = av(23456, [32], F32)
        end_r = av(23520, [32], F32)
        ntB = av(23584, [128], parts=32)
        tmp32 = av(23712, [16, 32], F32)
        sldef = av(24736, [64, 4], F32)
        widx = av(52800, [64], F32).bitcast(I32)
        self.issue_casts(None, layer=l)
        wkeys = [("wcast", l, q) for q in range(12)]
        if not hasattr(self, "_bc"):
            self._bc = {}

            def mk(e):
                self._bc["r"] = e.alloc_register("bcreg")
                return e.reg_mov(self._bc["r"], 4095)
            P.op("pool", mk)
        bc = self._bc
        P.op("dve", MSET(sldef, 0.0), writes=["sldef"])
        P.op("dve", MSET(sldef[:, :, 1:2], 6000.0), writes=["sldef"])
        P.op("sp", DMA(self.SL.rearrange("(p s) c -> p s c", s=64), sldef), reads=["sldef"], writes=["SL"], dma=True)
        ohb4 = av(23264, [4, 32])
        pbs = {}

        def frontA(j):
            tok = slice(j * 512, (j + 1) * 512)
            T0 = j * 4
            h2f, h2b = h2f_[j % 2], h2b_[j % 2]
            hk = j % 2
            srcs = [(self.xT[:, c, tok], ("xT", c, j)) for c in range(8)]
            dsts = [(h2f[:, c, :], ("h2f", hk, c)) for c in range(8)]
            scales = [self.scl2[:, l, c, bi:bi + 1] for c in range(8)]
            shifts = [self.modT[:, l, 24 + c, bi:bi + 1] for c in range(8)]

            def extra(c, dst, dkey, h2b=h2b, hk=hk):
                P.op("pool", CP(h2b[:, c, :], dst), reads=[dkey], writes=[("h2b", hk, c)])
            self.rmsnorm_T(srcs, 512, 0, scales, shifts, dsts, 7, extra)

        def frontB(j):
            T0 = j * 4
            h2f, h2b = h2f_[j % 2], h2b_[j % 2]
            hk = j % 2
            pb = 5 + self.rot("rt_ps", 2)
            psl = self.ps[pb]
            for t in range(4):
                T = T0 + t
                tt = slice(t * 128, (t + 1) * 128)
                p4 = 3 + self.rot("hrps", 2)
                ptb = self.ps[p4][:].bitcast(BF16)
                for kc in range(8):
                    P.op("pe", TR(ptb[:, kc * 128:(kc + 1) * 128], h2b[:, kc, tt], self.ident_b[:]),
                         reads=[("h2b", hk, kc), "ident_b"], writes=[("ps", p4)])
                hr = self.rot("hrow", 2)
                P.op("act", ACTV(hrow[hr], ptb[:, 0:1024], AF.Copy), reads=[("ps", p4)], writes=[("hrow", hr)])
                P.op("sp", DMA(self.hs[T * 128:(T + 1) * 128, :], hrow[hr]), reads=[("hrow", hr)], writes=[("hs", T)], dma=True)
                P.op("pe", MM(psl[:, t * 36:(t + 1) * 36], self.ones_f[0:1, :], self.rows_f[0:1, l * 36:(l + 1) * 36], True, False),
                     reads=["ones_f", "rows_f"], writes=[("ps", pb)])
                for kc in range(8):
                    P.op("pe", MM(psl[:, t * 36:(t + 1) * 36], h2f[:, kc, tt], self.wr[:, l, kc, :], False, kc == 7),
                         reads=[("h2f", hk, kc), "wr"], writes=[("ps", pb)])
            pbs[j] = pb

        def chain(j):
            T0 = j * 4
            pb = pbs[j]
            psl = self.ps[pb]
            k = "rt"
            lg = sm[:, 0:4, :]
            P.op("act", ACTV(lg, psl[:, 0:144].rearrange("p (t e) -> p t e", t=4), AF.Copy), reads=[("ps", pb)], writes=[k])
            gmax, gs, gw, m1 = sm[:, 4, 0:4], sm[:, 4, 4:8], sm[:, 4, 8:12], sm[:, 4, 12:16]
            m2, dn, w1, w2 = sm[:, 4, 16:20], sm[:, 4, 20:24], sm[:, 4, 24:28], sm[:, 4, 28:32]
            v3 = lambda ap: ap.rearrange("p (t g) -> p t g", t=4)
            ohg, pen, eg = v3(sm[:, 5, 0:16]), v3(sm[:, 5, 16:32]), v3(sm[:, 6, 0:16])
            elm, elm2 = sm[:, 7:11, 0:32], sm[:, 11:15, 0:32]
            oh1, oh2 = oh_all[:, T0:T0 + 4, 0, :], oh_all[:, T0:T0 + 4, 1, :]
            b4 = lambda ap, n: ap.unsqueeze(2).broadcast_to([128, 4, n])
            o = lambda eng, fn: P.op(eng, fn, reads=[k], writes=[k])
            o("dve", RMAX(gmax, lg[:, :, 0:4]))
            o("dve", TT(ohg, lg[:, :, 0:4], b4(gmax, 4), ALU.is_equal))
            o("dve", TT(eg, lg[:, :, 0:4], b4(gmax, 4), ALU.subtract))
            o("act", ACTV(eg, eg, AF.Exp))
            o("dve", RSUM(gs, eg))
            o("dve", lambda e: e.reciprocal(out=gw, in_=gs))
            o("dve", TS(pen, ohg, 1.0, 1.0e4, ALU.subtract, ALU.mult))
            o("dve", TT(elm.rearrange("p t (g e) -> p t g e", g=4), lg[:, :, 4:36].rearrange("p t (g e) -> p t g e", g=4),
                        pen.unsqueeze(3).broadcast_to([128, 4, 4, 8]), ALU.add))
            o("dve", RMAX(m1, elm))
            o("dve", TT(oh1, elm, b4(m1, 32), ALU.is_equal))
            o("dve", STT(elm2, oh1, -1.0e4, elm, ALU.mult, ALU.add))
            o("dve", RMAX(m2, elm2))
            o("dve", TT(oh2, elm2, b4(m2, 32), ALU.is_equal))
            o("dve", TT(dn, m2, m1, ALU.subtract))
            o("act", ACTV(w2, dn, AF.Sigmoid))
            o("act", ACTV(w1, dn, AF.Sigmoid, scale=-1.0))
            o("dve", TT(wts_all[:, T0:T0 + 4, 0], w1, gw, ALU.mult))
            o("dve", TT(wts_all[:, T0:T0 + 4, 1], w2, gw, ALU.mult))
            P.op("dve", TT(ohb4, oh1, oh2, ALU.add), reads=[k], writes=["ohb4"])
            for t in range(4):
                T = T0 + t
                outp = psl[:, 160 + t * 32:160 + (t + 1) * 32]
                last = (T == 0)
                P.op("pe", MM(outp, self.U_b[:], ohb4[:, t, :], True, last), reads=["U_b", "ohb4"], writes=[("ps", pb)])
                if j > 0:
                    P.op("pe", MM(outp, self.ones_b[:], ohcum, False, t == 0), reads=["ones_b", "ohcum"], writes=[("ps", pb)])
                for t2 in range(t):
                    P.op("pe", MM(outp, self.ones_b[:], ohb4[:, t2, :], False, t2 == t - 1), reads=["ones_b", "ohb4"], writes=[("ps", pb)])
            P.op("act", ACTV(pre_all[:, T0:T0 + 4, :], psl[:, 160:288].rearrange("p (t e) -> p t e", t=4), AF.Copy),
                 reads=[("ps", pb)], writes=[("pre", j)])
            bsum = sm[:, 15, 0:32]
            P.op("dve", RSUM(bsum, ohb4.rearrange("p t e -> p e t")), reads=["ohb4"], writes=["bsum"])
            if j == 0:
                P.op("dve", CP(ohcum, bsum), reads=["bsum"], writes=["ohcum"])
            else:
                P.op("dve", TT(ohcum, ohcum, bsum, ALU.add), reads=["bsum", "ohcum"], writes=["ohcum"])

        frontA(0)
        frontA(1)
        frontB(0)
        frontA(2)
        frontB(1)
        chain(0)
        frontA(3)
        frontB(2)
        chain(1)
        frontB(3)
        chain(2)
        chain(3)
        k = "rt"
        pn = self.ps[5]
        ntc, vv, fr = sm[0:32, 18, 0:1], sm[0:32, 18, 1:2], sm[0:32, 18, 2:3]
        P.op("pe", MM(pn[0:32, 0:1], ohcum, self.ones_b[:, 0:1], True, True), reads=["ohcum", "ones_b"], writes=[("ps", 5)])
        thr = sm[0:32, 16, 0:16]
        cmt = sm[0:32, 17, 0:16]
        P.op("dve", CP(vv, pn[0:32, 0:1]), reads=[("ps", 5)], writes=[k])
        P.op("dve", TS1(thr, iota_r[0:32, 0:16], 128.0, ALU.mult), reads=["iota"], writes=[k])
        P.op("dve", TS1(cmt, thr, vv, ALU.is_lt), reads=[k], writes=[k])
        P.op("dve", RSUM(ntc, cmt), reads=[k], writes=[k])
        P.op("dve", CP(ntB, ntc.broadcast_to([32, 128])), reads=[k], writes=["ntB"])
        pbb = self.ps[6]
        P.op("pe", MM(pbb[:, 0:32], ntB, self.U_b[0:32, 0:32], True, True), reads=["ntB", "U_b"], writes=[("ps", 6)])
        P.op("pe", MM(pbb[:, 32:64], ntB, self.ident_b[0:32, 0:32], True, True), reads=["ntB", "ident_b"], writes=[("ps", 6)])
        P.op("act", ACTV(base_r, pbb[:, 0:32], AF.Copy), reads=[("ps", 6)], writes=["base_r"])
        P.op("act", ACTV(nt_r, pbb[:, 32:64], AF.Copy), reads=[("ps", 6)], writes=["nt_r"])
        P.op("dve", TT(end_r, base_r, nt_r, ALU.add), reads=["base_r", "nt_r"], writes=["end_r"])
        P.op("dve", STT(pos_all, base_r.unsqueeze(1).broadcast_to([128, 16, 32]), 128.0, pre_all, ALU.mult, ALU.add),
             reads=["base_r"] + [("pre", jj) for jj in range(4)], writes=["pos_all"])
        for r in range(2):
            P.op("dve", TT(tmp32, oh_all[:, :, r, :], pos_all, ALU.mult), reads=["rt", "pos_all"], writes=["tmp32"])
            P.op("dve", RSUM(slot_f[:, :, r], tmp32), reads=["tmp32"], writes=["slot_f"])
            P.op("dve", TS(vals_all[:, :, r, 0], iota_r[:, 0:16], 128.0, iota_p, ALU.mult, ALU.add), reads=["iota"], writes=["vals"])
            P.op("dve", TS(vals_all[:, :, r, 1], vals_all[:, :, r, 0], 1.0, float(r * S), ALU.mult, ALU.add), reads=["vals"], writes=["vals"])
            P.op("dve", CP(vals_all[:, :, r, 2], wts_all[:, :, r]), reads=["rt"], writes=["vals"])
            P.op("dve", MSET(vals_all[:, :, r, 3], 0.0), writes=["vals"])
        P.op("dve", CP(slot_i, slot_f), reads=["slot_f"], writes=["slot_i"])
        for T in range(16):
            for r in range(2):
                P.op("pool", (lambda e, T=T, r=r: e.indirect_dma_start(
                    out=self.SL, out_offset=bass.IndirectOffsetOnAxis(ap=slot_i[:, T, r:r + 1], axis=0),
                    in_=vals_all[:, T, r, :], in_offset=None)), reads=["slot_i", "vals", "SL"], writes=[("SLw", T, r)], dma=True)
        P.op("dve", TT(cmp, end_r.unsqueeze(1).broadcast_to([128, 64, 32]), iota_r.unsqueeze(2).broadcast_to([128, 64, 32]), ALU.is_le),
             reads=["end_r", "iota"], writes=["cmp"])
        P.op("dve", RSUM(te, cmp), reads=["cmp"], writes=["te"])
        P.op("dve", TS(te, te, 128.0, iota_p, ALU.mult, ALU.add), reads=["te", "iota"], writes=["te"])
        P.op("dve", CP(widx, te), reads=["te"], writes=["widx"])
        P.barrier()
        NBUF = 6
        wgu_sb = [av(i * 4096, [8, 512]) for i in range(NBUF)]
        wd_sb = [av(24576 + i * 2048, [2, 1024]) for i in range(NBUF)]
        hg = [av(36864 + i * 1024, [1024]) for i in range(NBUF)]
        hgT = [av(43008 + i * 1024, [8, 128]) for i in range(2)]
        a_sb = [av(45056 + i * 256, [256]) for i in range(2)]
        aT = [av(45568 + i * 256, [2, 128]) for i in range(2)]
        osb = [av(46080 + i * 2048, [1024], F32) for i in range(2)]
        SLall = av(50176, [64, 4], F32)
        sidx_all = av(50688, [64, 2], F32).bitcast(I32)
        P.op("sp", DMA(SLall, self.SL.rearrange("(j p) c -> p j c", p=128)), writes=["SLall"], dma=True)
        P.op("dve", CP(sidx_all, SLall[:, :, 0:2]), reads=["SLall"], writes=["sidx_all"])

        def loads(jt):
            r3 = jt % NBUF
            P.op("pool", (lambda e: e.indirect_dma_start(
                out=wgu_sb[r3].rearrange("p a b -> p (a b)"), out_offset=None, in_=self.wgu_b[l],
                in_offset=bass.IndirectOffsetOnAxis(ap=widx[:, jt:jt + 1], axis=0), bounds_check=bc["r"], oob_is_err=False)),
                reads=["widx"] + wkeys, writes=[("wgu", r3)], dma=True)
            P.op("pool", (lambda e: e.indirect_dma_start(
                out=wd_sb[r3].rearrange("p a b -> p (a b)"), out_offset=None, in_=self.wd_b[l],
                in_offset=bass.IndirectOffsetOnAxis(ap=widx[:, jt:jt + 1], axis=0), bounds_check=bc["r"], oob_is_err=False)),
                reads=["widx"] + wkeys, writes=[("wd", r3)], dma=True)
            P.op("pool", (lambda e: e.indirect_dma_start(
                out=hg[r3], out_offset=None, in_=self.hs, in_offset=bass.IndirectOffsetOnAxis(ap=sidx_all[:, jt, 0:1], axis=0))),
                reads=["sidx_all"], writes=[("hg", r3)], dma=True)

        for q in range(NBUF):
            loads(q)

        def S1(jt):
            r2, r3 = jt % 2, jt % NBUF
            pt = 0 + r2
            ptb = self.ps[pt][:].bitcast(BF16)
            for kc in range(8):
                P.op("pe", TR(ptb[:, kc * 128:(kc + 1) * 128], hg[r3][:, kc * 128:(kc + 1) * 128], self.ident_b[:]),
                     reads=[("hg", r3), "ident_b"], writes=[("ps", pt)])
            P.op("act", ACTV(hgT[r2], ptb[:, 0:1024].rearrange("p (a b) -> p a b", a=8), AF.Copy),
                 reads=[("ps", pt)], writes=[("hgT", r2)])

        def S2(jt):
            r2, r3 = jt % 2, jt % NBUF
            pg = 2 + r2
            for kc in range(8):
                P.op("pe", MM(self.ps[pg][:], hgT[r2][:, kc, :], wgu_sb[r3][:, kc, :], kc == 0, kc == 7),
                     reads=[("hgT", r2), ("wgu", r3)], writes=[("ps", pg)])
            sg = self.tmpf[r2]
            P.op("act", ACTV(sg[:, 0:256], self.ps[pg][:, 0:256], AF.Silu), reads=[("ps", pg)], writes=[("tmpf", r2)])
            P.op("dve", STT(a_sb[r2], sg[:, 0:256], SLall[:, jt, 2:3], self.ps[pg][:, 256:512], ALU.mult, ALU.mult),
                 reads=[("tmpf", r2), "SLall", ("ps", pg)], writes=[("a", r2)])

        def S3(jt):
            r2 = jt % 2
            pt = 6 + r2
            ptb = self.ps[pt][:].bitcast(BF16)
            for c in range(2):
                P.op("pe", TR(ptb[:, c * 128:(c + 1) * 128], a_sb[r2][:, c * 128:(c + 1) * 128], self.ident_b[:]),
                     reads=[("a", r2), "ident_b"], writes=[("ps", pt)])
            P.op("act", ACTV(aT[r2], ptb[:, 0:256].rearrange("p (a b) -> p a b", a=2), AF.Copy),
                 reads=[("ps", pt)], writes=[("aT", r2)])

        def S4(jt):
            r2, r3 = jt % 2, jt % NBUF
            for hf in range(2):
                po = 4 + hf
                for kc in range(2):
                    P.op("pe", MM(self.ps[po][:], aT[r2][:, kc, :], wd_sb[r3][:, kc, hf * 512:(hf + 1) * 512], kc == 0, kc == 1),
                         reads=[("aT", r2), ("wd", r3)], writes=[("ps", po)])
                P.op("act" if hf == 0 else "dve", (ACTV(osb[r2][:, 0:512], self.ps[po][:], AF.Copy) if hf == 0 else
                                                   CP(osb[r2][:, 512:1024], self.ps[po][:])),
                     reads=[("ps", po)], writes=[("osb", r2, hf)])
            P.op("pool", (lambda e: e.indirect_dma_start(
                out=self.Y, out_offset=bass.IndirectOffsetOnAxis(ap=sidx_all[:, jt, 1:2], axis=0), in_=osb[r2], in_offset=None,
                bounds_check=bc["r"], oob_is_err=False)),
                reads=["sidx_all", ("osb", r2, 0), ("osb", r2, 1)], writes=[("Y", jt)], dma=True)
            if jt + NBUF < NT + 0 and jt + NBUF - 1 < NT:
                pass

        for k in range(NT + 3):
            if k < NT:
                S1(k)
            if 0 <= k - 1 < NT:
                S2(k - 1)
            if 0 <= k - 2 < NT:
                S3(k - 2)
            if 0 <= k - 3 < NT:
                S4(k - 3)
                if (k - 3) + NBUF < NT:
                    loads((k - 3) + NBUF)
        P.barrier()
        ysum = [av(i * 2048, [1024], F32) for i in range(4)]
        ytmp = [av(8192 + i * 2048, [1024], F32) for i in range(2)]
        for j in range(4):
            for t in range(4):
                T = j * 4 + t
                yt = self.rot("ytmp", 2)
                P.op("sp", DMA(ysum[t], self.Y[T * 128:(T + 1) * 128, :]), writes=[("ysum", t)], dma=True)
                P.op("sp", DMA(ytmp[yt], self.Y[S + T * 128:S + (T + 1) * 128, :]), writes=[("ytmp", yt)], dma=True)
                P.op("dve", TT(ysum[t], ysum[t], ytmp[yt], ALU.add), reads=[("ysum", t), ("ytmp", yt)], writes=[("ysum", t)])
            for m in range(8):
                pc = self.rot("cps", 4)
                for t in range(4):
                    P.op("pe", TR(self.ps[pc][:, t * 128:(t + 1) * 128], ysum[t][:, m * 128:(m + 1) * 128], self.ident_f[:]),
                         reads=[("ysum", t), "ident_f"], writes=[("ps", pc)])
                tok = slice(j * 512, (j + 1) * 512)
                P.op("dve", STT(self.xT[:, m, tok], self.ps[pc][:], g2(m), self.xT[:, m, tok], ALU.mult, ALU.add),
                     reads=[("ps", pc), ("xT", m, j), "modT"], writes=[("xT", m, j)])

    def phase_mix1(self, bi):
        P, d = self.P, self.d
        l = 1
        hT = self.av(0, [8, 512])
        wsl = [self.av(4096 + s * 4096, [8, 512]) for s in range(3)]
        wos = [self.av(16384 + s * 4096, [16, 256]) for s in range(2)]
        vn = self.av(24576, [4, 2048])
        gT = self.av(32768, [16, 512])
        lngB = self.av(40960, [1, 2048])[:, 0]
        lnbB = self.av(43008, [1, 2048])[:, 0]
        wsT = self.av(45056, [8, 128])
        rowsb = self.av(46080, [1, 3072], parts=1)[:, 0]
        P.op("pool", DMA(rowsb, d["rows"][:, R_CBV:R_CBV + 3072]), writes=["rows_b"], dma=True)
        sm = self.sm
        g1 = lambda m: self.modT[:, l, 16 + m, bi:bi + 1]
        if self.sparse:
            while self.cast_jobs and self.cast_jobs[0][3][0] == "cwcast":
                self.issue_casts(1)
            cw_in, cw_out, cwk = self.cwin_b, self.cwout_b, [("cwcast", q) for q in range(4)]
        else:
            cw_in, cw_out, cwk = d["c_w_in"], d["c_w_out"], []
        P.op("pool", DMA(lngB, d["c_ln_g"][0].partition_broadcast(128)), writes=["lngB"], dma=True)
        P.op("pool", DMA(lnbB, d["c_ln_b"][0].partition_broadcast(128)), writes=["lnbB"], dma=True)
        P.op("pool", DMA(wsT, d["w_sT"]), writes=["wsT"], dma=True)
        for j in range(4):
            tok = slice(j * 512, (j + 1) * 512)
            srcs = [(self.xT[:, c, tok], ("xT", c, j)) for c in range(8)]
            dsts = [(hT[:, c, :], ("hT", c)) for c in range(8)]
            scales = [self.scl1[:, l, c, bi:bi + 1] for c in range(8)]
            shifts = [self.modT[:, l, 0 + c, bi:bi + 1] for c in range(8)]
            self.rmsnorm_T(srcs, 512, 0, scales, shifts, dsts, 7)
            for s in range(4):
                slot = self.rot("wsl", 3)
                w = wsl[slot]
                P.op("pool", DMA(w, cw_in[:, 2048 + s * 512:2048 + (s + 1) * 512].rearrange("(kc p) n -> p kc n", p=128)),
                     reads=cwk, writes=[("wsl", slot)], dma=True)
                for t in range(4):
                    pb = self.rot("m1ps", 4)
                    ps = self.ps[pb]
                    P.op("pe", MM(ps[:], self.ones_b[0:1, :], rowsb[0:1, s * 512:(s + 1) * 512], True, False),
                         reads=["ones_b", "rows_b"], writes=[("ps", pb)])
                    for kc in range(8):
                        P.op("pe", MM(ps[:], hT[:, kc, t * 128:(t + 1) * 128], w[:, kc, :], False, kc == 7),
                             reads=[("hT", kc), ("wsl", slot)], writes=[("ps", pb)])
                    P.op("act", ACTV(vn[:, t, s * 512:(s + 1) * 512], ps[:], AF.Gelu_apprx_tanh),
                         reads=[("ps", pb)], writes=[("vn", t, s)])
                    P.op("dve", lambda e, t=t, s=s: e.bn_stats(out=sm[:, 8 + t, s * 6:(s + 1) * 6], in_=vn[:, t, s * 512:(s + 1) * 512]),
                         reads=[("vn", t, s)], writes=[("bst", t)])
            for t in range(4):
                mv = sm[:, 12 + t, 0:2]
                rstd, nmr = sm[:, 12 + t, 2:3], sm[:, 12 + t, 3:4]
                vk = [("vn", t, s) for s in range(4)]
                P.op("dve", lambda e, t=t, mv=mv: e.bn_aggr(out=mv, in_=sm[:, 8 + t, 0:24]), reads=[("bst", t)], writes=[("mv", t)])
                P.op("act", ACTV(rstd, mv[:, 1:2], AF.Sqrt, bias=self.cst[:, 4:5]), reads=[("mv", t), "cst"], writes=[("mv", t)])
                P.op("dve", lambda e, rstd=rstd: e.reciprocal(out=rstd, in_=rstd), reads=[("mv", t)], writes=[("mv", t)])
                P.op("dve", STT(nmr, mv[:, 0:1], -1.0, rstd, ALU.mult, ALU.mult), reads=[("mv", t)], writes=[("mv", t)])
                P.op("dve", TS(vn[:, t, :], vn[:, t, :], rstd, nmr, ALU.mult, ALU.add), reads=vk + [("mv", t)], writes=vk)
                P.op("dve", TT(vn[:, t, :], vn[:, t, :], lngB, ALU.mult), reads=vk + ["lngB"], writes=vk)
                P.op("dve", TT(vn[:, t, :], vn[:, t, :], lnbB, ALU.add), reads=vk + ["lnbB"], writes=vk)
            for su in range(4):
                slot = self.rot("wsl", 3)
                w = wsl[slot]
                P.op("pool", DMA(w, cw_in[:, su * 512:(su + 1) * 512].rearrange("(kc p) n -> p kc n", p=128)),
                     reads=cwk, writes=[("wsl", slot)], dma=True)
                for q in range(4):
                    cc = su * 4 + q
                    g = cc // 2
                    pu = self.rot("m1ps", 4)
                    for kc in range(8):
                        P.op("pe", MM(self.ps[pu][:], w[:, kc, q * 128:(q + 1) * 128], hT[:, kc, :], kc == 0, kc == 7),
                             reads=[("wsl", slot), ("hT", kc)], writes=[("ps", pu)])
                    rb = self.rot("tmpb", 4)
                    uT = self.tmpb[rb]
                    P.op("act", ACTV(uT[:], self.ps[pu][:], AF.Gelu_apprx_tanh, bias=self.vecs[:, V_CBU + cc:V_CBU + cc + 1]),
                         reads=[("ps", pu), "vecs"], writes=[("tmpb", rb)])
                    pz = 4 + self.rot("m1pz", 2)
                    pss = self.ps[pz]
                    P.op("pe", MM(pss[:].rearrange("p (a b) -> p a b", a=4), self.ones_b[0:1, :],
                                  rowsb[0:1, 2048 + g * 128:2048 + (g + 1) * 128].unsqueeze(1).broadcast_to([1, 4, 128]),
                                  True, False), reads=["ones_b", "rows_b"], writes=[("ps", pz)])
                    for n in range(4):
                        P.op("pe", MM(pss[:, n * 128:(n + 1) * 128], vn[:, n, cc * 128:(cc + 1) * 128], wsT[:, g, :], False, n == 3),
                             reads=[("vn", n, cc // 4), "wsT"], writes=[("ps", pz)])
                    P.op("dve", TT(gT[:, cc, :], uT[:], pss[:], ALU.mult), reads=[("tmpb", rb), ("ps", pz)], writes=[("gT", cc)])
            for so in range(4):
                slot = self.rot("wos", 2)
                wo = wos[slot]
                P.op("pool", DMA(wo, cw_out[:, so * 256:(so + 1) * 256].rearrange("(kc p) n -> p kc n", p=128)),
                     reads=cwk, writes=[("wos", slot)], dma=True)
                for mm in range(2):
                    m = so * 2 + mm
                    py = 6 + self.rot("py", 2)
                    for kc in range(16):
                        P.op("pe", MM(self.ps[py][:], wo[:, kc, mm * 128:(mm + 1) * 128], gT[:, kc, :], kc == 0, kc == 15),
                             reads=[("wos", slot), ("gT", kc)], writes=[("ps", py)])
                    P.op("dve", STT(self.xT[:, m, tok], self.ps[py][:], g1(m), self.xT[:, m, tok], ALU.mult, ALU.add),
                         reads=[("ps", py), ("xT", m, j), "modT"], writes=[("xT", m, j)])

    def phase_mix0(self, bi):
        P, d = self.P, self.d
        l = 0
        sm = self.sm
        g1 = lambda m: self.modT[:, l, 16 + m, bi:bi + 1]
        qlat = self.av(0, [3, S])
        kvlat = self.av(6144, [2, S + CTX])
        krt = self.av(10752, [18, 32], F32)
        R0 = 11904
        hT = self.av(R0, [8, S + CTX])
        w_in = self.av(30336, [8, 1696])
        zT = self.av(43904, [4, S + 30])
        ctxs = self.av(43904, [8, CTX], F32)
        for (c0, c1) in ((0, 384), (384, 672), (672, 1184), (1184, 1696)):
            P.op("pool", DMA(w_in[:, :, c0:c1], d["a_w_in"][:, c0:c1].rearrange("(kc p) n -> p kc n", p=128)),
                 writes=[("w_in", c0)], dma=True)
        wkey = lambda col: ("w_in", 0 if col < 384 else 384 if col < 672 else 672 if col < 1184 else 1184)
        P.op("sp", DMA(ctxs, d["ctxT"][bi].rearrange("(kc p) n -> p kc n", p=128)), writes=["ctxs"], dma=True)
        srcs = [(ctxs[:, c, :], "ctxs") for c in range(8)]
        dsts = [(hT[:, c, S:S + CTX], ("hT", c, 4)) for c in range(8)]
        self.rmsnorm_T(srcs, CTX, 0, [self.scl1[:, l, c, 2:3] for c in range(8)],
                       [self.modT[:, l, c, 2:3] for c in range(8)], dsts, 7)
        for j in range(4):
            tok = slice(j * 512, (j + 1) * 512)
            srcs = [(self.xT[:, c, tok], ("xT", c, j)) for c in range(8)]
            dsts = [(hT[:, c, tok], ("hT", c, j)) for c in range(8)]
            self.rmsnorm_T(srcs, 512, 0, [self.scl1[:, l, c, bi:bi + 1] for c in range(8)],
                           [self.modT[:, l, c, bi:bi + 1] for c in range(8)], dsts, 7)
        self.issue_casts(10)
        for c in range(4):
            P.op("dve", MSET(zT[:, c, 0:15], 0.0), writes=[("zT", c, "p0")])
            P.op("dve", MSET(zT[:, c, S + 15:S + 30], 0.0), writes=[("zT", c, "p1")])
        for j in range(5):
            ntok = 512 if j < 4 else CTX
            tok = slice(j * 512, j * 512 + ntok)
            hk = lambda kc: ("hT", kc, j)
            if j < 4:
                srcs = []
                for c in range(3):
                    for kc in range(8):
                        P.op("pe", MM(self.ps[c][:, 0:ntok], w_in[:, kc, c * 128:(c + 1) * 128], hT[:, kc, tok], kc == 0, kc == 7),
                             reads=[wkey(c * 128), hk(kc)], writes=[("ps", c)])
                    srcs.append((self.ps[c][:, 0:ntok], ("ps", c)))
                self.rmsnorm_T(srcs, ntok, 1, [self.qkng[:, c:c + 1] for c in range(3)], None,
                               [(qlat[:, c, tok], ("qlat", c, j)) for c in range(3)], 7)
            srcs = []
            for c in range(2):
                for kc in range(8):
                    P.op("pe", MM(self.ps[3 + c][:, 0:ntok], w_in[:, kc, 384 + c * 128:384 + (c + 1) * 128], hT[:, kc, tok], kc == 0, kc == 7),
                         reads=[wkey(384), hk(kc)], writes=[("ps", 3 + c)])
                srcs.append((self.ps[3 + c][:, 0:ntok], ("ps", 3 + c)))
            self.rmsnorm_T(srcs, ntok, 2, [self.qkng[:, 3 + c:4 + c] for c in range(2)], None,
                           [(kvlat[:, c, tok], ("kvlat", c, j)) for c in range(2)], 7)
            PKR, PGL = _Rec(), _Rec()
            for t in range(ntok // 128):
                T = j * 4 + t
                pb = 5 + self.rot("krps", 2)
                for kc in range(8):
                    PKR.op("pe", MM(self.ps[pb][:, 0:32], hT[:, kc, j * 512 + t * 128:j * 512 + (t + 1) * 128], w_in[:, kc, 640:672], kc == 0, kc == 7),
                         reads=[wkey(640), hk(kc)], writes=[("ps", pb)])
                k = "krw"
                raw, sqr, kg = sm[:, 16, 0:32], sm[:, 17, 0:32], sm[:, 18, 0:32]
                t1, t2 = sm[:, 19, 0:16], sm[:, 19, 16:32]
                o = lambda eng, fn: PKR.op(eng, fn, reads=[k, "kgB", "rope"], writes=[k])
                PKR.op("act", ACTV(raw, self.ps[pb][:, 0:32], AF.Copy), reads=[("ps", pb)], writes=[k])
                o("dve", TT(sqr, raw, raw, ALU.mult))
                PKR.op("dve", RSUM(sm[:, 20, T:T + 1], sqr), reads=[k], writes=[k, ("sskr", T)])
                if j < 4:
                    o("dve", TT(kg, raw, self.kgB[:, 64:96], ALU.mult))
                    cs, sn = self.rope[:, T, 0:16], self.rope[:, T, 16:32]
                    o("dve", TT(t1, kg[:, 0:16], cs, ALU.mult))
                    o("dve", TT(t2, kg[:, 16:32], sn, ALU.mult))
                    PKR.op("dve", TT(krt[:, T, 0:16], t1, t2, ALU.subtract), reads=[k], writes=[k, ("krt", T)])
                    o("dve", TT(t1, kg[:, 0:16], sn, ALU.mult))
                    o("dve", TT(t2, kg[:, 16:32], cs, ALU.mult))
                    PKR.op("dve", TT(krt[:, T, 16:32], t1, t2, ALU.add), reads=[k], writes=[k, ("krt", T)])
                else:
                    PKR.op("dve", TT(krt[:, T, :], raw, self.kgB[:, 64:96], ALU.mult), reads=[k, "kgB"], writes=[k, ("krt", T)])
            if j < 4:
                for c in range(4):
                    r = self.rot("glu", 2)
                    pa, pbk = 0 + r, 2 + r
                    for kc in range(8):
                        PGL.op("pe", MM(self.ps[pa][:], w_in[:, kc, 672 + c * 128:672 + (c + 1) * 128], hT[:, kc, tok], kc == 0, kc == 7),
                             reads=[wkey(672), hk(kc)], writes=[("ps", pa)])
                    for kc in range(8):
                        PGL.op("pe", MM(self.ps[pbk][:], w_in[:, kc, 1184 + c * 128:1184 + (c + 1) * 128], hT[:, kc, tok], kc == 0, kc == 7),
                             reads=[wkey(1184), hk(kc)], writes=[("ps", pbk)])
                    rf = self.rot("tmpf", 2)
                    PGL.op("act", ACTV(self.tmpf[rf][:], self.ps[pbk][:], AF.Sigmoid), reads=[("ps", pbk)], writes=[("tmpf", rf)])
                    PGL.op("dve", TT(zT[:, c, 15 + j * 512:15 + (j + 1) * 512], self.ps[pa][:], self.tmpf[rf][:], ALU.mult),
                         reads=[("ps", pa), ("tmpf", rf)], writes=[("zT", c, j)])
            for i_ in range(max(len(PKR.l), len(PGL.l))):
                if i_ < len(PGL.l):
                    P.op(*PGL.l[i_][0], **PGL.l[i_][1])
                if i_ < len(PKR.l):
                    P.op(*PKR.l[i_][0], **PKR.l[i_][1])
        P.barrier()
        Dg = self.av(R0, [4, 31, 128])
        convT = self.av(27776, [4, S])
        w_oc = self.av(35968, [4, D])
        bdw = self.av(40064, [1, 512], parts=1)[:, 0]
        P.op("pool", DMA(bdw, d["rows"][:, R_BDW:R_BDW + 512]), writes=["bdw"], dma=True)
        P.op("pool", DMA(w_oc, d["ab_w_out"][512:1024, :].rearrange("(kc p) n -> p kc n", p=128)), writes=["w_oc"], dma=True)
        self.issue_casts(6)
        for c in range(4):
            P.op("dve", TT(Dg[:, c, :, :], self.ident_b[:].unsqueeze(1).broadcast_to([128, 31, 128]),
                           self.vecs[:, V_DW + c * 31:V_DW + (c + 1) * 31].unsqueeze(2).broadcast_to([128, 31, 128]), ALU.mult),
                 reads=["ident_b", "vecs"], writes=[("Dg", c)])
        zkeys = lambda c: [("zT", c, "p0"), ("zT", c, "p1")] + [("zT", c, jj) for jj in range(4)]
        cst_ = {}

        def convA(t):
            pc = 0 + self.rot("convps", 2)
            ps = self.ps[pc]
            P.op("pe", MM(ps[:], self.ones_b[0:1, :], bdw[0:1, :], True, False), reads=["ones_b", "bdw"], writes=[("ps", pc)])
            for c in range(4):
                for jt in range(31):
                    P.op("pe", MM(ps[:, c * 128:(c + 1) * 128], zT[:, c, t * 128 + jt:t * 128 + jt + 128], Dg[:, c, jt, :],
                                  False, c == 3 and jt == 30), reads=zkeys(c) + [("Dg", c)], writes=[("ps", pc)])
            k = ("cst", t % 2)
            st6, mv = sm[:, 21 + t % 2, 0:6], sm[:, 21 + t % 2, 6:8]
            rstd, nmr = sm[:, 21 + t % 2, 8:9], sm[:, 21 + t % 2, 9:10]
            P.op("dve", lambda e, st6=st6, ps=ps: e.bn_stats(out=st6, in_=ps[:]), reads=[("ps", pc)], writes=[k])
            P.op("dve", lambda e, st6=st6, mv=mv: e.bn_aggr(out=mv, in_=st6), reads=[k], writes=[k])
            P.op("act", ACTV(rstd, mv[:, 1:2], AF.Sqrt, bias=self.cst[:, 4:5]), reads=[k, "cst"], writes=[k])
            P.op("dve", lambda e, rstd=rstd: e.reciprocal(out=rstd, in_=rstd), reads=[k], writes=[k])
            P.op("dve", STT(nmr, mv[:, 0:1], -1.0, rstd, ALU.mult, ALU.mult), reads=[k], writes=[k])
            rb = self.rot("tmpb", 4)
            cn = self.tmpb[rb]
            P.op("act", ACTV(cn[:], ps[:], AF.Identity, bias=nmr, scale=rstd), reads=[k, ("ps", pc)], writes=[("tmpb", rb)])
            cst_[t] = (rb, cn)

        def convB(t):
            rb, cn = cst_[t]
            pt = 2 + self.rot("convpt", 2)
            ptb = self.ps[pt][:].bitcast(BF16)
            for c in range(4):
                P.op("pe", TR(ptb[:, c * 128:(c + 1) * 128], cn[:, c * 128:(c + 1) * 128], self.ident_b[:]),
                     reads=[("tmpb", rb), "ident_b"], writes=[("ps", pt)])
            for c in range(4):
                P.op("act", ACTV(convT[:, c, t * 128:(t + 1) * 128], ptb[:, c * 128:(c + 1) * 128], AF.Silu,
                                 bias=self.vecs[:, V_BLNB + c:V_BLNB + c + 1], scale=self.vecs[:, V_BLNG + c:V_BLNG + c + 1]),
                     reads=[("ps", pt), "vecs"], writes=[("convT", c, t // 4)])

        convA(0)
        for t in range(1, 16):
            convA(t)
            convB(t - 1)
        convB(15)
        for j in range(4):
            tok = slice(j * 512, (j + 1) * 512)
            for m in range(8):
                py = 4 + self.rot("py0", 2)
                for kc in range(4):
                    P.op("pe", MM(self.ps[py][:], w_oc[:, kc, m * 128:(m + 1) * 128], convT[:, kc, tok], kc == 0, kc == 3),
                         reads=["w_oc", ("convT", kc, j)], writes=[("ps", py)])
                P.op("dve", STT(self.xT[:, m, tok], self.ps[py][:], g1(m), self.xT[:, m, tok], ALU.mult, ALU.add),
                     reads=[("ps", py), ("xT", m, j), "modT"], writes=[("xT", m, j)])
        P.barrier()
        QT = self.av(R0, [4, S], parts=96)
        KT = self.av(20096, [4, S + CTX], parts=96)
        Vaug = self.av(29312, [18, 4, 128])
        attnT = self.av(38528, [4, S])
        w_uq = self.av(46720, [3, 384])
        w_ukv = self.av(47872, [2, 512])
        w_oa = self.av(48896, [4, D])
        P.op("pool", DMA(w_oa, d["ab_w_out"][0:512, :].rearrange("(kc p) n -> p kc n", p=128)), writes=["w_oa"], dma=True)
        for hh in range(2):
            if hh == 1:
                P.barrier()
            P.op("pool", DMA(w_uq, d["a_w_uq"][:, hh * 384:(hh + 1) * 384].rearrange("(kc p) n -> p kc n", p=128)), writes=["w_uq"], dma=True)
            P.op("pool", DMA(w_ukv, d["a_w_ukv"][:, hh * 512:(hh + 1) * 512].rearrange("(kc p) n -> p kc n", p=128)), writes=["w_ukv"], dma=True)
            self.issue_casts(6)
            P.op("dve", MSET(Vaug[:, :, :, :], 1.0), writes=[("V", T) for T in range(18)])
            for T in range(18):
                tk = slice(T * 128, (T + 1) * 128)
                J = min(T // 4, 4)
                PK, PQ = _Rec(), _Rec()
                pk = 0 + self.rot("kvps", 2)
                psk = self.ps[pk]
                for kc in range(2):
                    PK.op("pe", MM(psk[:], kvlat[:, kc, tk], w_ukv[:, kc, :], kc == 0, kc == 1),
                         reads=[("kvlat", kc, J), "w_ukv"], writes=[("ps", pk)])
                psk3 = psk[:].rearrange("p (h e) -> p h e", h=4)
                k = "kw"
                sqf = self.tmpf[0][:, 256:512].rearrange("p (h e) -> p h e", h=4)
                ssk, rk = sm[:, 23, 0:4], sm[:, 23, 4:8]
                kn = self.tmpf[0][:, 0:256].rearrange("p (h e) -> p h e", h=4)
                PK.op("act", ACTV(sqf, psk3[:, :, 0:64], AF.Square), reads=[("ps", pk)], writes=[("tmpf", 0)])
                PK.op("dve", RSUM(ssk, sqf), reads=[("tmpf", 0)], writes=[k])
                PK.op("dve", TS1(ssk, ssk, sm[:, 20, T:T + 1], ALU.add), reads=[k, ("sskr", T)], writes=[k])
                PK.op("act", ACTV(rk, ssk, AF.Sqrt, bias=self.cst[:, 3:4]), reads=[k, "cst"], writes=[k])
                PK.op("dve", lambda e, rk=rk: e.reciprocal(out=rk, in_=rk), reads=[k], writes=[k])
                PK.op("dve", TT(kn, psk3[:, :, 0:64], rk.unsqueeze(2).broadcast_to([128, 4, 64]), ALU.mult),
                     reads=[("ps", pk), k], writes=[("tmpf", 0)])
                PK.op("dve", TT(self.kfin[:, :, 0:64], kn, self.kgB[:, 0:64].unsqueeze(1).broadcast_to([128, 4, 64]), ALU.mult),
                     reads=[("tmpf", 0), "kgB"], writes=["kfin"])
                PK.op("dve", TT(self.kfin[:, :, 64:96], krt[:, T, :].unsqueeze(1).broadcast_to([128, 4, 32]),
                               rk.unsqueeze(2).broadcast_to([128, 4, 32]), ALU.mult), reads=[("krt", T), k], writes=["kfin"])
                for par in range(2):
                    PK.op("act", ACTV(Vaug[:, T, par:4:2, par * 64:par * 64 + 64], psk3[:, par:4:2, 64:128], AF.Copy),
                         reads=[("ps", pk)], writes=[("V", T)])
                ptk = 4
                ptb = self.ps[ptk][:].bitcast(BF16)
                for hl in range(4):
                    PK.op("pe", TR(ptb[0:96, hl * 128:(hl + 1) * 128], self.kfin[:, hl, :], self.ident_b[:]),
                         reads=["kfin", "ident_b"], writes=[("ps", ptk)])
                PK.op("act", ACTV(KT[:, :, tk], ptb[0:96, 0:512].rearrange("p (h e) -> p h e", h=4), AF.Copy),
                     reads=[("ps", ptk)], writes=[("KT", T)])
                if T < 16:
                    pq = 2 + self.rot("qps", 2)
                    psq = self.ps[pq]
                    for kc in range(3):
                        PQ.op("pe", MM(psq[:, 0:384], qlat[:, kc, tk], w_uq[:, kc, :], kc == 0, kc == 2),
                             reads=[("qlat", kc, J), "w_uq"], writes=[("ps", pq)])
                    psq3 = psq[:, 0:384].rearrange("p (h e) -> p h e", h=4)
                    k = "qw"
                    sqq = self.rstdB[:, 0:384].rearrange("p (h e) -> p h e", h=4)
                    ssq, rq = sm[:, 23, 8:12], sm[:, 23, 12:16]
                    qn = self.tmpf[1][:, 0:384].rearrange("p (h e) -> p h e", h=4)
                    ta = sm[:, 16, :].bitcast(F32) if False else None
                    PQ.op("act", ACTV(sqq, psq3, AF.Square), reads=[("ps", pq)], writes=["rstdB"])
                    PQ.op("dve", RSUM(ssq, sqq), reads=["rstdB"], writes=[k])
                    PQ.op("act", ACTV(rq, ssq, AF.Sqrt, bias=self.cst[:, 3:4]), reads=[k, "cst"], writes=[k])
                    PQ.op("dve", lambda e, rq=rq: e.reciprocal(out=rq, in_=rq), reads=[k], writes=[k])
                    PQ.op("dve", TT(qn, psq3, rq.unsqueeze(2).broadcast_to([128, 4, 96]), ALU.mult),
                         reads=[("ps", pq), k], writes=[("tmpf", 1)])
                    PQ.op("dve", TT(qn, qn, self.qgB[:].unsqueeze(1).broadcast_to([128, 4, 96]), ALU.mult),
                         reads=[("tmpf", 1), "qgB"], writes=[("tmpf", 1)])
                    PQ.op("act", ACTV(self.qfin[:, :, 0:64], qn[:, :, 0:64], AF.Copy), reads=[("tmpf", 1)], writes=["qfin"])
                    cs = self.rope[:, T, 0:16].unsqueeze(1).broadcast_to([128, 4, 16])
                    sn = self.rope[:, T, 16:32].unsqueeze(1).broadcast_to([128, 4, 16])
                    u1 = sm[:, 0, 0:64].rearrange("p (h e) -> p h e", h=4) if False else self.tmpf[1][:, 384:448].rearrange("p (h e) -> p h e", h=4)
                    u2 = self.tmpf[1][:, 448:512].rearrange("p (h e) -> p h e", h=4)
                    kq = ("tmpf", 1)
                    PQ.op("dve", TT(u1, qn[:, :, 64:80], cs, ALU.mult), reads=[("tmpf", 1), "rope"], writes=[kq])
                    PQ.op("dve", TT(u2, qn[:, :, 80:96], sn, ALU.mult), reads=[("tmpf", 1), "rope"], writes=[kq])
                    PQ.op("dve", TT(self.qfin[:, :, 64:80], u1, u2, ALU.subtract), reads=[kq], writes=["qfin"])
                    PQ.op("dve", TT(u1, qn[:, :, 64:80], sn, ALU.mult), reads=[("tmpf", 1), "rope"], writes=[kq])
                    PQ.op("dve", TT(u2, qn[:, :, 80:96], cs, ALU.mult), reads=[("tmpf", 1), "rope"], writes=[kq])
                    PQ.op("dve", TT(self.qfin[:, :, 80:96], u1, u2, ALU.add), reads=[kq], writes=["qfin"])
                    ptq = 5
                    ptb = self.ps[ptq][:].bitcast(BF16)
                    for hl in range(4):
                        PQ.op("pe", TR(ptb[0:96, hl * 128:(hl + 1) * 128], self.qfin[:, hl, :], self.ident_b[:]),
                             reads=["qfin", "ident_b"], writes=[("ps", ptq)])
                    PQ.op("act", ACTV(QT[:, :, tk], ptb[0:96, 0:512].rearrange("p (h e) -> p h e", h=4), AF.Copy),
                         reads=[("ps", ptq)], writes=[("QT", T // 4)])

                for i_ in range(max(len(PK.l), len(PQ.l))):
                    if i_ < len(PK.l):
                        P.op(*PK.l[i_][0], **PK.l[i_][1])
                    if i_ < len(PQ.l):
                        P.op(*PQ.l[i_][0], **PQ.l[i_][1])
            for hl in range(4):
                h = hh * 4 + hl
                par = hl % 2
                orow = slice(par * 64, par * 64 + 64)
                drow = slice((1 - par) * 64, (1 - par) * 64 + 64)
                for jq in range(4):
                    tq = slice(jq * 512, (jq + 1) * 512)
                    po = 6 + self.rot("po", 2)
                    pso = self.ps[po]
                    pend = None
                    for kp in range(10):
                        if kp < 9:
                            pr = self.rot("sps2", 2)
                            b0 = 2 * pr
                            for q in range(2):
                                kt = 2 * kp + q
                                P.op("pe", MM(self.ps[b0 + q][:], KT[:, hl, kt * 128:(kt + 1) * 128], QT[:, hl, tq], True, True),
                                     reads=[("KT", kt), ("QT", jq)], writes=[("ps", b0 + q)])
                            rb = self.rot("tmpb2", 2)
                            P.op("act", ACTV(self.tmpb2[rb][:], self.psall[:, b0 * 512:(b0 + 2) * 512], AF.Exp,
                                             bias=self.negC[:], scale=math.sqrt(96.0)),
                                 reads=[("ps", b0), ("ps", b0 + 1), "negC"], writes=[("tmpb", 2 * rb), ("tmpb", 2 * rb + 1)])
                        if pend is not None:
                            pkp, prb = pend
                            for q in range(2):
                                pkt = 2 * pkp + q
                                P.op("pe", MM(pso[:], Vaug[:, pkt, hl, :], self.tmpb2[prb][:, q * 512:(q + 1) * 512], pkt == 0, pkt == 17),
                                     reads=[("V", pkt), ("tmpb", 2 * prb + q)], writes=[("ps", po)])
                        pend = (kp, rb) if kp < 9 else None
                    rf = self.rot("tmpf", 2)
                    rden = self.tmpf[rf]
                    P.op("dve", lambda e, rden=rden, pso=pso, orow=orow, drow=drow: e.reciprocal(out=rden[orow, :], in_=pso[drow, :]),
                         reads=[("ps", po)], writes=[("tmpf", rf)])
                    P.op("dve", TT(attnT[orow, h // 2, tq], pso[orow, :], rden[orow, :], ALU.mult),
                         reads=[("ps", po), ("tmpf", rf)], writes=[("attnT", h // 2, jq)])
        for j in range(4):
            tok = slice(j * 512, (j + 1) * 512)
            for m in range(8):
                py = 4 + self.rot("py0", 2)
                for kc in range(4):
                    P.op("pe", MM(self.ps[py][:], w_oa[:, kc, m * 128:(m + 1) * 128], attnT[:, kc, tok], kc == 0, kc == 3),
                         reads=["w_oa", ("attnT", kc, j)], writes=[("ps", py)])
                P.op("dve", STT(self.xT[:, m, tok], self.ps[py][:], g1(m), self.xT[:, m, tok], ALU.mult, ALU.add),
                     reads=[("ps", py), ("xT", m, j), "modT"], writes=[("xT", m, j)])


def _rope_tables():
    t = np.arange(S)
    row = (t // 64).astype(np.float32)
    col = (t % 64).astype(np.float32)
    inv = (np.float32(10000.0) ** (-np.arange(8, dtype=np.float32) / np.float32(8))).astype(np.float32)
    ang = np.concatenate([row[:, None] * inv, col[:, None] * inv], axis=-1).astype(np.float32)
    cs = np.concatenate([np.cos(ang), np.sin(ang)], axis=-1).astype(np.float32)
    return np.ascontiguousarray(cs.reshape(16, 128, 32).transpose(1, 0, 2))


def _chunks(v):
    v = np.asarray(v, np.float32).reshape(-1, 128)
    return v.T


def make_in_maps(inp, ncores=NCORES, nb=NB, sparse=False):
    f = lambda a: np.ascontiguousarray(np.asarray(a, np.float32))
    shared = dict(
        rope=_rope_tables(), ident=np.eye(128, dtype=np.float32),
        w_ada=f(inp["w_ada"]), a_w_in=f(inp["a_w_in"][0]), a_w_uq=f(inp["a_w_uq"][0]), a_w_ukv=f(inp["a_w_ukv"][0]),
        a_q_g=f(inp["a_q_g"]), a_k_g=f(inp["a_k_g"]), b_w_dw=f(inp["b_w_dw"][0]), ab_w_out=f(inp["ab_w_out"][0]),
        c_w_in=f(inp["c_w_in"][0]), c_ln_g=f(inp["c_ln_g"]), c_ln_b=f(inp["c_ln_b"]),
        w_sT=f(np.transpose(inp["c_w_s"][0], (2, 0, 1))), c_w_out=f(inp["c_w_out"][0]),
        wr=f(np.concatenate([inp["moe_w_group"], np.transpose(inp["moe_w_router"], (0, 2, 1, 3)).reshape(2, D, 32)], axis=-1)),
    )
    if sparse:
        g = np.asarray(inp["moe_w_gate"], np.float32).reshape(2, 32, 8, 128, 256)
        u = np.asarray(inp["moe_w_up"], np.float32).reshape(2, 32, 8, 128, 256)
        gu = np.concatenate([g, u], axis=-1)
        gu = np.ascontiguousarray(np.transpose(gu, (0, 1, 3, 2, 4))).reshape(2, 4096, 4096)
        shared["wgu_t0"], shared["wgu_t1"] = gu[0], gu[1]
        dn = np.asarray(inp["moe_w_down"], np.float32).reshape(2, 32, 2, 128, D)
        dn = np.ascontiguousarray(np.transpose(dn, (0, 1, 3, 2, 4))).reshape(2, 4096, 2048)
        shared["wd_t0"], shared["wd_t1"] = dn[0], dn[1]
        cst = np.zeros((128, 193), np.float32)
        cst[:, 0:64] = np.arange(64, dtype=np.float32)[None, :]
        cst[:, 64] = np.arange(128, dtype=np.float32)
        cst[:, 65:193] = np.triu(np.ones((128, 128), np.float32), 1)
        shared["consts"] = cst
    else:
        shared.update(moe_w_gate=f(inp["moe_w_gate"]), moe_w_up=f(inp["moe_w_up"]), moe_w_down=f(inp["moe_w_down"]))
    rows = np.zeros((1, NR), np.float32)
    rows[0, R_BDW:R_BDW + 512] = inp["b_b_dw"][0]
    rows[0, R_CBV:R_CBV + 2048] = inp["c_b_in"][0][2048:]
    rows[0, R_BS:R_BS + 1024] = np.asarray(inp["c_b_s"][0]).reshape(-1)
    for l in range(2):
        rows[0, R_MOEB + l * 36:R_MOEB + l * 36 + 4] = inp["moe_b_group"][l]
        rows[0, R_MOEB + l * 36 + 4:R_MOEB + (l + 1) * 36] = np.asarray(inp["moe_b_router"][l]).reshape(-1)
    shared["rows"] = rows
    maps = []
    for ci in range(ncores):
        bs = [ci * nb + k for k in range(nb)]
        vecs = np.zeros((128, NV), np.float32)
        cc = np.zeros((128, 8, 3), np.float32)
        for k, b in enumerate(bs):
            cc[:, :, k] = _chunks(inp["c"][b])
        cc[:, :, 2] = _chunks(inp["c_ctx"])
        if nb == 1:
            cc[:, :, 1] = cc[:, :, 0]
        vecs[:, V_C:V_C + 24] = cc.reshape(128, 24)
        for l in range(2):
            vecs[:, V_BADA + l * 48:V_BADA + (l + 1) * 48] = _chunks(inp["b_ada"][l])
            vecs[:, V_N1G + l * 8:V_N1G + (l + 1) * 8] = _chunks(inp["norm1_g"][l])
            vecs[:, V_N2G + l * 8:V_N2G + (l + 1) * 8] = _chunks(inp["norm2_g"][l])
        vecs[:, V_QNG:V_QNG + 3] = _chunks(inp["a_q_norm_g"][0])
        vecs[:, V_KVNG:V_KVNG + 2] = _chunks(inp["a_kv_norm_g"][0])
        vecs[:, V_BLNG:V_BLNG + 4] = _chunks(inp["b_ln_g"][0])
        vecs[:, V_BLNB:V_BLNB + 4] = _chunks(inp["b_ln_b"][0])
        vecs[:, V_CBU:V_CBU + 16] = _chunks(inp["c_b_in"][0][:2048])
        vecs[:, V_DW:V_DW + 124] = np.transpose(np.asarray(inp["b_w_dw"][0], np.float32).reshape(31, 4, 128), (2, 1, 0)).reshape(128, 124)
        m = dict(shared)
        m["vecs"] = vecs
        m["xT"] = np.ascontiguousarray(np.transpose(np.asarray(inp["x"], np.float32)[bs], (0, 2, 1)))
        m["ctxT"] = np.ascontiguousarray(np.transpose(np.asarray(inp["ctx"], np.float32)[bs], (0, 2, 1)))
        maps.append(m)
    return maps


_PROG_CACHE = {}


def get_prog(phases, nb):
    key = (tuple(phases), nb)
    if key not in _PROG_CACHE:
        _PROG_CACHE[key] = KB(list(phases), nb).nc
    return _PROG_CACHE[key]


def kernel(**inputs):
    phases = ["mix0", "smoe0", "mix1", "smoe1"]
    nc = get_prog(phases, NB)
    maps = make_in_maps(inputs, sparse=True)
    res = run_bass_kernel_spmd(nc, maps, core_ids=list(range(NCORES)))
    out = np.empty((16, S, D), np.float32)
    for ci in range(NCORES):
        o = res.results[ci]["outT"]
        for k in range(NB):
            out[ci * NB + k] = o[k].T
    return out
```
